# Optimizing a Trainium2 kernel written in Bass

```python
import jax
import jax.numpy as jnp
from jax import lax
import numpy as np

D_MODEL = 1024
BATCH = 4
SEQ = 8192
DEPTH = 2

GRID_W = 64
CTX_LEN = 256
EPS = 1e-6
N_MOD = 6

F_GROUPS = 4
F_GROUP_DIM = 64
F_WIDTH = F_GROUPS * F_GROUP_DIM
NA_HEADS = 8
NA_HEAD_DIM = 64
NA_WIDTH = NA_HEADS * NA_HEAD_DIM
WIN_H = 8
WIN_W = 16
Q_BLOCK = 16
KEY_SPAN = Q_BLOCK + WIN_W
CONV_GROUPS = 4
CONV_GROUP_DIM = 64
CONV_WIDTH = CONV_GROUPS * CONV_GROUP_DIM
CONV_K = 3
N_BRANCH = 3

Q_OFF = F_WIDTH
K_OFF = Q_OFF + NA_WIDTH
V_OFF = K_OFF + NA_WIDTH
U_OFF = V_OFF + NA_WIDTH
B_OFF = U_OFF + CONV_WIDTH
C_OFF = B_OFF + CONV_WIDTH
G_OFF = C_OFF + CONV_WIDTH
PROJ_WIDTH = G_OFF + N_BRANCH * D_MODEL
SPLITS = (Q_OFF, K_OFF, V_OFF, U_OFF, B_OFF, C_OFF, G_OFF)

D_FF = 2816
N_EXPERTS = 8
TOP_K = 2
D_FF_EXPERT = 3584
N_DENSE = (DEPTH + 1) // 2
N_MOE = DEPTH // 2

kernel_name = "hybrid_fnet_natten_shortconv_moe_dit"


def rmsnorm(x, g):
    xf = x.astype(jnp.float32)
    y = xf * lax.rsqrt(jnp.mean(xf * xf, axis=-1, keepdims=True) + EPS)
    return (y * g.astype(jnp.float32)).astype(x.dtype)


def modulate(h, shift, scale):
    return h * (1 + scale) + shift


def heads(t):
    return t.reshape(t.shape[0], t.shape[1], NA_HEADS, NA_HEAD_DIM)


def fourier_mix(u):
    b, l, _ = u.shape
    uf = u.astype(jnp.float32).reshape(b, l, F_GROUPS, F_GROUP_DIM)
    y = jnp.fft.fft2(uf, axes=(1, 3), norm="ortho").real
    return y.reshape(b, l, F_WIDTH).astype(u.dtype)


def short_conv(u, w):
    return lax.conv_general_dilated(
        u, w[:, None, :].astype(u.dtype), window_strides=(1,),
        padding=[(CONV_K // 2, CONV_K // 2)],
        dimension_numbers=("NWC", "WIO", "NWC"),
        feature_group_count=u.shape[-1])


def context_attention(q, k, v):
    s = jnp.einsum("blhd,bmhd->bhlm", q, k).astype(jnp.float32) * (NA_HEAD_DIM ** -0.5)
    p = jax.nn.softmax(s, axis=-1).astype(v.dtype)
    o = jnp.einsum("bhlm,bmhd->blhd", p, v)
    return o.reshape(o.shape[0], o.shape[1], NA_WIDTH)


def neighbourhood_attention(q, k, v, k_ctx, v_ctx, rpb):
    b, s_len, h, dh = q.shape
    rows = s_len // GRID_W
    kh = min(WIN_H, rows)
    qg = q.reshape(b, rows, GRID_W, h, dh)
    kg = k.reshape(b, rows, GRID_W, h, dh)
    vg = v.reshape(b, rows, GRID_W, h, dh)
    r = jnp.arange(rows)
    row_idx = jnp.clip(r - kh // 2, 0, rows - kh)[:, None] + jnp.arange(kh)[None, :]
    k_rows = kg[:, row_idx]
    v_rows = vg[:, row_idx]
    dr = row_idx - r[:, None] + (WIN_H - 1)
    col_start = np.clip(np.arange(GRID_W) - WIN_W // 2, 0, GRID_W - WIN_W)
    scale = NA_HEAD_DIM ** -0.5
    n_win = kh * KEY_SPAN
    outs = []
    for a in range(0, GRID_W, Q_BLOCK):
        s0 = min(max(a - WIN_W // 2, 0), GRID_W - KEY_SPAN)
        qc = np.arange(a, a + Q_BLOCK)
        kc = np.arange(s0, s0 + KEY_SPAN)
        cs = col_start[qc][:, None]
        in_win = (kc[None, :] >= cs) & (kc[None, :] < cs + WIN_W)
        dc = np.clip(kc[None, :] - qc[:, None] + (WIN_W - 1), 0, 2 * WIN_W - 2)
        qb = qg[:, :, a:a + Q_BLOCK]
        kb = k_rows[:, :, :, s0:s0 + KEY_SPAN]
        vb = v_rows[:, :, :, s0:s0 + KEY_SPAN]
        s_win = jnp.einsum("brqhd,brkwhd->bhrqkw", qb, kb).astype(jnp.float32) * scale
        bias = rpb[:, dr[:, None, :, None], dc[None, :, None, :]]
        s_win = s_win + bias.astype(jnp.float32)[None]
        s_win = jnp.where(in_win[:, None, :], s_win, -jnp.inf)
        s_ctx = jnp.einsum("brqhd,blhd->bhrql", qb, k_ctx).astype(jnp.float32) * scale
        s_all = jnp.concatenate([s_win.reshape(b, h, rows, Q_BLOCK, n_win), s_ctx], axis=-1)
        p = jax.nn.softmax(s_all, axis=-1).astype(v.dtype)
        p_win = p[..., :n_win].reshape(b, h, rows, Q_BLOCK, kh, KEY_SPAN)
        p_ctx = p[..., n_win:]
        o = (jnp.einsum("bhrqkw,brkwhd->brqhd", p_win, vb)
             + jnp.einsum("bhrql,blhd->brqhd", p_ctx, v_ctx))
        outs.append(o)
    out = jnp.concatenate(outs, axis=2)
    return out.reshape(b, s_len, NA_WIDTH)


def merge_branches(zf, zu, zb, zc, zg, attn, conv_w, w_fourier, w_na, w_conv_out, w_out):
    y_f = fourier_mix(zf) @ w_fourier
    y_a = attn @ w_na
    y_c = (zb * short_conv(zc * zu, conv_w)) @ w_conv_out
    g_f, g_a, g_c = jnp.split(jax.nn.sigmoid(zg), N_BRANCH, axis=-1)
    return (g_f * y_f + g_a * y_a + g_c * y_c) @ w_out


def swiglu(h, w_gate, w_up, w_down):
    return (jax.nn.silu(h @ w_gate) * (h @ w_up)) @ w_down


def moe(h, w_router, w_gate, w_up, w_down):
    logits = (h @ w_router).astype(jnp.float32)
    top_val, top_idx = lax.top_k(logits, TOP_K)
    top_w = jax.nn.softmax(top_val, axis=-1)
    gates = jnp.sum(jax.nn.one_hot(top_idx, N_EXPERTS, dtype=jnp.float32) * top_w[..., None],
                    axis=-2).astype(h.dtype)
    out = jnp.zeros_like(h)
    for e in range(N_EXPERTS):
        out = out + gates[..., e:e + 1] * swiglu(h, w_gate[e], w_up[e], w_down[e])
    return out


def setup_inputs(seed: int = 0) -> dict:
    key = jax.random.key(seed)
    ks = jax.random.split(key, 24)
    f32 = jnp.float32
    D = D_MODEL

    def nrm(k, shape, scale):
        return jax.random.normal(k, shape, f32) * scale

    return {
        "x": nrm(ks[0], (BATCH, SEQ, D), 1.0),
        "c": nrm(ks[1], (BATCH, D), 1.0),
        "ctx": nrm(ks[2], (BATCH, CTX_LEN, D), 1.0),
        "c_ctx": nrm(ks[3], (D,), 1.0),
        "norm1_g": 1.0 + nrm(ks[4], (DEPTH, D), 0.1),
        "norm2_g": 1.0 + nrm(ks[5], (DEPTH, D), 0.1),
        "w_ada": nrm(ks[6], (DEPTH, D, N_MOD * D), 0.5 * D ** -0.5),
        "b_ada": nrm(ks[7], (DEPTH, N_MOD * D), 0.01),
        "w_in": nrm(ks[8], (DEPTH, D, PROJ_WIDTH), D ** -0.5),
        "conv_w": nrm(ks[9], (DEPTH, CONV_K, CONV_WIDTH), CONV_K ** -0.5),
        "na_rpb": nrm(ks[10], (DEPTH, NA_HEADS, 2 * WIN_H - 1, 2 * WIN_W - 1), 0.1),
        "w_fourier": nrm(ks[11], (DEPTH, F_WIDTH, D), F_WIDTH ** -0.5),
        "w_na": nrm(ks[12], (DEPTH, NA_WIDTH, D), NA_WIDTH ** -0.5),
        "w_conv_out": nrm(ks[13], (DEPTH, CONV_WIDTH, D), CONV_WIDTH ** -0.5),
        "w_out": nrm(ks[14], (DEPTH, D, D), D ** -0.5),
        "ffn_w_gate": nrm(ks[15], (N_DENSE, D, D_FF), D ** -0.5),
        "ffn_w_up": nrm(ks[16], (N_DENSE, D, D_FF), D ** -0.5),
        "ffn_w_down": nrm(ks[17], (N_DENSE, D_FF, D), D_FF ** -0.5),
        "moe_router": nrm(ks[18], (N_MOE, D, N_EXPERTS), D ** -0.5),
        "moe_w_gate": nrm(ks[19], (N_MOE, N_EXPERTS, D, D_FF_EXPERT), D ** -0.5),
        "moe_w_up": nrm(ks[20], (N_MOE, N_EXPERTS, D, D_FF_EXPERT), D ** -0.5),
        "moe_w_down": nrm(ks[21], (N_MOE, N_EXPERTS, D_FF_EXPERT, D), D_FF_EXPERT ** -0.5),
        "final_g": 1.0 + nrm(ks[22], (D,), 0.1),
    }


def reference(x, c, ctx, c_ctx, norm1_g, norm2_g, w_ada, b_ada, w_in, conv_w, na_rpb,
              w_fourier, w_na, w_conv_out, w_out, ffn_w_gate, ffn_w_up, ffn_w_down,
              moe_router, moe_w_gate, moe_w_up, moe_w_down, final_g):
    def channel_mixer(l, h):
        if l % 2 == 0:
            j = l // 2
            return swiglu(h, ffn_w_gate[j], ffn_w_up[j], ffn_w_down[j])
        j = l // 2
        return moe(h, moe_router[j], moe_w_gate[j], moe_w_up[j], moe_w_down[j])

    for l in range(DEPTH):
        last = l == DEPTH - 1
        mod_x = [m[:, None, :] for m in jnp.split(jax.nn.silu(c) @ w_ada[l] + b_ada[l], N_MOD, axis=-1)]
        mod_c = jnp.split(jax.nn.silu(c_ctx) @ w_ada[l] + b_ada[l], N_MOD, axis=-1)

        hx = modulate(rmsnorm(x, norm1_g[l]), mod_x[0], mod_x[1])
        hc = modulate(rmsnorm(ctx, norm1_g[l]), mod_c[0], mod_c[1])
        zf, zq, zk, zv, zu, zb, zcg, zg = jnp.split(hx @ w_in[l], SPLITS, axis=-1)

        if last:
            k_c, v_c = jnp.split(hc @ w_in[l][:, K_OFF:U_OFF], 2, axis=-1)
            k_c, v_c = heads(k_c), heads(v_c)
        else:
            cf, cq, ck, cv, cu, cb, ccg, cg = jnp.split(hc @ w_in[l], SPLITS, axis=-1)
            k_c, v_c = heads(ck), heads(cv)
            attn_c = context_attention(heads(cq), k_c, v_c)
            mix_c = merge_branches(cf, cu, cb, ccg, cg, attn_c, conv_w[l],
                                   w_fourier[l], w_na[l], w_conv_out[l], w_out[l])
            ctx_mid = ctx + mod_c[2] * mix_c
            hc2 = modulate(rmsnorm(ctx_mid, norm2_g[l]), mod_c[3], mod_c[4])
            ctx = ctx_mid + mod_c[5] * channel_mixer(l, hc2)

        attn_x = neighbourhood_attention(heads(zq), heads(zk), heads(zv), k_c, v_c, na_rpb[l])
        mix_x = merge_branches(zf, zu, zb, zcg, zg, attn_x, conv_w[l],
                               w_fourier[l], w_na[l], w_conv_out[l], w_out[l])
        x = x + mod_x[2] * mix_x

        hx2 = modulate(rmsnorm(x, norm2_g[l]), mod_x[3], mod_x[4])
        x = x + mod_x[5] * channel_mixer(l, hx2)

    return rmsnorm(x, final_g)
```

```python
import numpy as np
import ml_dtypes
from contextlib import ExitStack
import concourse.bass as bass
import concourse.mybir as mybir
from concourse.bass_utils import run_bass_kernel_spmd

F32 = mybir.dt.float32
BF16 = mybir.dt.bfloat16
ALU = mybir.AluOpType
AF = mybir.ActivationFunctionType

ENGS = ("pe", "act", "dve", "pool", "sp")
EPOCH = 30000
NDMA = 14

D = 1024
T = 8192
TC = 256
TT = T + TC
PW = 5632
NEG = -30000.0
PADR = 512
CT0 = PADR + T + PADR
XB_ROWS = CT0 + TC
HALF = T // 2
LT = HALF + 2 * PADR


def xbrow(tok):
    return PADR + tok if tok < T else CT0 + (tok - T)


class Res:
    __slots__ = ("name", "w", "r", "multi")

    def __init__(self, name="", multi=False):
        self.name = name
        self.w = {}
        self.r = {}
        self.multi = multi


class Sched:
    def __init__(self, nc, es):
        self.nc = nc
        self.es = es
        self.q = {e: [] for e in ENGS}
        self.cnt = {e: 0 for e in ENGS}
        self.seen = {e: {} for e in ENGS}
        self.sems = {}
        self.dma_i = {e: 0 for e in ENGS}
        self.last = {}

    def sem(self, key):
        s = self.sems.get(key)
        if s is None:
            s = self.es.enter_context(self.nc.semaphore("s_%s" % "_".join(str(k) for k in key)))
            self.sems[key] = s
        return s

    def _deps(self, eng, reads, writes):
        deps = {}

        def add(k, v):
            if deps.get(k, 0) < v:
                deps[k] = v
        for r in reads:
            for k, v in r.w.items():
                add(k, v)
        for w in writes:
            for k, v in w.r.items():
                add(k, v)
            if (not w.multi) or w.r:
                for k, v in w.w.items():
                    add(k, v)
        out = []
        seen = self.seen[eng]
        for k, v in deps.items():
            if seen.get(k, 0) < v:
                seen[k] = v
                out.append((self.sem(k), v))
        return out

    def _mark(self, tok, reads, writes):
        k, v = tok
        self.last[k] = max(self.last.get(k, 0), v)
        for r in reads:
            if r.r.get(k, 0) < v:
                r.r[k] = v
        for w in writes:
            if w.multi and not w.r:
                if w.w.get(k, 0) < v:
                    w.w[k] = v
            else:
                w.w = {k: v}
                w.r = {}

    def op(self, eng, fns, reads=(), writes=()):
        if not isinstance(fns, (list, tuple)):
            fns = [fns]
        waits = self._deps(eng, reads, writes)
        self.cnt[eng] += 1
        if self.cnt[eng] % EPOCH == 0:
            self.cnt[eng] += 1
        n = self.cnt[eng]
        key = (eng, n // EPOCH)
        val = n % EPOCH
        self.q[eng].append((waits, fns, self.sem(key), 1))
        tok = (key, val)
        self._mark(tok, reads, writes)
        return tok

    def dma(self, eng, out, in_, reads=(), writes=(), **kw):
        i = self.dma_i[eng]
        self.dma_i[eng] += 1
        slot = i % NDMA
        key = ("dma", eng, slot)
        val = 16 * (i // NDMA + 1)
        sem = self.sem(key)
        waits = self._deps(eng, reads, writes)
        if i >= NDMA:
            pv = 16 * (i // NDMA)
            if self.seen[eng].get(key, 0) < pv:
                self.seen[eng][key] = pv
                waits.append((sem, pv))
        fn = lambda e, out=out, in_=in_, kw=kw: e.dma_start(out=out, in_=in_, **kw)
        self.q[eng].append((waits, [fn], sem, 16))
        tok = (key, val)
        self._mark(tok, reads, writes)
        return tok

    def dma_fn(self, eng, fn, reads=(), writes=()):
        i = self.dma_i[eng]
        self.dma_i[eng] += 1
        slot = i % NDMA
        key = ("dma", eng, slot)
        val = 16 * (i // NDMA + 1)
        sem = self.sem(key)
        waits = self._deps(eng, reads, writes)
        if i >= NDMA:
            pv = 16 * (i // NDMA)
            if self.seen[eng].get(key, 0) < pv:
                self.seen[eng][key] = pv
                waits.append((sem, pv))
        self.q[eng].append((waits, [fn], sem, 16))
        tok = (key, val)
        self._mark(tok, reads, writes)
        return tok

    def barrier(self):
        for eng in ENGS:
            waits = []
            for k, v in self.last.items():
                if self.seen[eng].get(k, 0) < v:
                    self.seen[eng][k] = v
                    waits.append((self.sem(k), v))
            if waits:
                self.q[eng].append((waits, [], None, 0))

    def emit(self):
        with self.nc.Block() as block:
            def runner(name):
                def run(e):
                    for waits, fns, sem, inc in self.q[name]:
                        for s, v in waits:
                            e.wait_ge(s, v)
                        ins = None
                        for f in fns:
                            ins = f(e)
                        if ins is not None and sem is not None:
                            ins.then_inc(sem, inc)
                return run
            block.tensor(runner("pe"))
            block.scalar(runner("act"))
            block.vector(runner("dve"))
            block.gpsimd(runner("pool"))
            block.sync(runner("sp"))


class Ring:
    def __init__(self, items):
        self.items = items
        self.i = 0

    def next(self):
        it = self.items[self.i % len(self.items)]
        self.i += 1
        return it


ARENA_BYTES = 184 * 1024


class Arena:
    def __init__(self, nc, es):
        self.t = es.enter_context(nc.sbuf_tensor("arena", [128, ARENA_BYTES // 2], BF16))
        self.off = 0

    def reset(self):
        self.off = 0

    def alloc(self, shape, dt, parts=128):
        n = 1
        for s in shape:
            n *= s
        four = dt in (F32, mybir.dt.int32, mybir.dt.uint32)
        nb = n * (4 if four else 2)
        nb_al = (nb + 63) // 64 * 64
        assert self.off + nb_al <= ARENA_BYTES, ("arena overflow", self.off, nb_al)
        e0 = self.off // 2
        ap = self.t[0:parts, e0:e0 + nb // 2]
        if four:
            ap = ap.bitcast(dt)
        self.off += nb_al
        if len(shape) == 1:
            return ap
        names = " ".join("d%d" % i for i in range(len(shape)))
        kw = {"d%d" % i: shape[i] for i in range(len(shape))}
        return ap.rearrange("p (%s) -> p %s" % (names, names), **kw)


class _StopBuild(Exception):
    pass


def build_program(dbg=False, stop=None, dumps=()):
    nc = bass.Bass("TRN2", target_bir_lowering=False)

    def din(name, shape, dt=F32):
        return nc.dram_tensor(name, list(shape), dt, kind="ExternalInput").ap()

    def dscr(name, shape, dt=BF16):
        return nc.dram_tensor(name, list(shape), dt).ap()

    x_in = din("x", [T, D])
    ctx_in = din("ctx", [TC, D])
    cvec = din("cvec", [2, D])
    norm1_g = din("norm1_g", [2, D])
    norm2_g = din("norm2_g", [2, D])
    w_ada = din("w_ada", [2, D, 6 * D])
    b_ada = din("b_ada", [2, 6 * D])
    w_in = din("w_in", [2, D, PW])
    conv_w = din("conv_w", [2, 3, 256])
    w_fourier = din("w_fourier", [2, 256, D])
    w_na = din("w_na", [2, 512, D])
    w_conv_out = din("w_conv_out", [2, 256, D])
    w_out = din("w_out", [2, D, D])
    ffn_wg = din("ffn_w_gate", [1, D, 2816])
    ffn_wu = din("ffn_w_up", [1, D, 2816])
    ffn_wd = din("ffn_w_down", [1, 2816, D])
    moe_router = din("moe_router", [1, D, 8])
    moe_wg = din("moe_w_gate", [1, 8, D, 3584])
    moe_wu = din("moe_w_up", [1, 8, D, 3584])
    moe_wd = din("moe_w_down", [1, 8, 3584, D])
    final_g = din("final_g", [1, D])
    biasT = din("biasT", [5, 128, 8 * 5 * 128], BF16)
    biasT1 = din("biasT1", [5, 128, 8 * 6 * 128], BF16)
    edgemask = din("edgemask", [1, 2])
    dftA = din("dftA", [128, 64 * 2 * 128], BF16)
    dft64 = din("dft64", [64, 3 * 64], BF16)
    dftc = din("dftc", [256, 2 * 256], BF16)
    c64bd = din("c64bd", [256, 2 * 256], BF16)
    y_out = nc.dram_tensor("y", [HALF, D], F32, kind="ExternalOutput").ap()

    xa = dscr("xa", [TT, D], F32)
    xb = dscr("xb", [XB_ROWS, D], F32)
    Ud = dscr("Ud", [TT, 256])
    qTd = dscr("qTd", [512, TT])
    kTd = dscr("kTd", [512, TT])
    vd = dscr("vd", [TT, 520])
    pTd = dscr("pTd", [256, TT])
    bTd = dscr("bTd", [256, TT])
    gTd = dscr("gTd", [3072, TT])
    Gd = dscr("Gd", [64, 128, 512])
    XTd = dscr("XTd", [512, TT])
    attnTd = dscr("attnTd", [512, TT])
    convTd = dscr("convTd", [256, TT])
    h2Td = dscr("h2Td", [D, TT])
    xloc = dscr("xloc", [LT, D], F32)
    XTloc = dscr("XTloc", [512, HALF])
    modd = dscr("modd", [2, 6 * D], F32)
    wrT = dscr("wrT", [8, D], F32)
    gatesd = dscr("gatesd", [T, 8], F32)
    fwg = dscr("fwg", [11, 128, 8 * 256])
    fwu = dscr("fwu", [11, 128, 8 * 256])
    fwd = dscr("fwd", [2816, D])
    mwgu = [dscr("mwgu%d" % q, [8 * D, 1792]) for q in range(4)]
    NGRP = 23
    Hs = dscr("Hs", [NGRP * 512, D])
    Ys = dscr("Ys", [NGRP * 512, D], F32)
    h2tok = dscr("h2tok", [HALF, D])
    mwd = dscr("mwd", [8, 3584, D])
    dbgs = {}
    scr_by_name = dict(xa=xa, xb=xb, Ud=Ud, qTd=qTd, kTd=kTd, vd=vd, pTd=pTd, bTd=bTd, gTd=gTd, Gd=Gd, XTd=XTd, attnTd=attnTd,
                       convTd=convTd, h2Td=h2Td, modd=modd, wrT=wrT, gatesd=gatesd, fwd=fwd, mwd=mwd, Hs=Hs, Ys=Ys, h2tok=h2tok)
    dump_out = {}
    for dn in dumps:
        src_ = scr_by_name[dn]
        dump_out[dn] = nc.dram_tensor("dump_" + dn, list(src_.shape), src_.dtype, kind="ExternalOutput").ap()
    if dbg:
        dbgs["xb1"] = nc.dram_tensor("dbg_xb1", [XB_ROWS, D], F32, kind="ExternalOutput").ap()
        dbgs["xa0"] = nc.dram_tensor("dbg_xa0", [TT, D], F32, kind="ExternalOutput").ap()

    with ExitStack() as es:
        S = Sched(nc, es)
        AR = Arena(nc, es)

        def sbt(name, shape, dt):
            return es.enter_context(nc.sbuf_tensor(name, shape, dt))

        identf = sbt("identf", [128, 128], F32)
        ident = sbt("ident", [128, 128], BF16)
        epsb = sbt("epsb", [128, 1], F32)
        scT = sbt("scT", [128, 8, 2], BF16)
        r_const = Res("const")
        Gall = sbt("Gall", [128, 32, 8], F32)
        r_Gall = Res("Gall", multi=True)
        I32 = mybir.dt.int32
        U32 = mybir.dt.uint32
        d12i = sbt("d12i", [128, 2, 32], I32)
        g12 = sbt("g12", [128, 2, 32], F32)
        r_route = Res("route", multi=True)
        r_h2tok = Res("h2tok", multi=True)
        psf = [(es.enter_context(nc.psum_tensor("psf%d" % i, [128, 512], F32)), Res("psf%d" % i)) for i in range(6)]
        psb = [(es.enter_context(nc.psum_tensor("psb%d" % i, [128, 1024], BF16)), Res("psb%d" % i)) for i in range(2)]
        PF = Ring(psf)
        PB = Ring(psb)
        evac_rr = [0]

        def evac_eng():
            evac_rr[0] += 1
            return "act" if evac_rr[0] % 2 else "dve"


        def f_mm(o, l, r, st, sp):
            return lambda e: e.matmul(o, lhsT=l, rhs=r, start=st, stop=sp)

        def f_tr(o, i):
            return lambda e: e.transpose(o, i, ident[:])

        def f_act(o, i, func, **kw):
            return lambda e: e.activation(out=o, in_=i, func=func, **kw)

        def f_tt(o, a, b, op):
            return lambda e: e.tensor_tensor(out=o, in0=a, in1=b, op=op)

        def f_ts(o, a, s1, op0):
            return lambda e: e.tensor_scalar(out=o, in0=a, scalar1=s1, scalar2=None, op0=op0)

        def f_stt(o, a, sc, b, op0, op1, **kw):
            return lambda e: e.scalar_tensor_tensor(out=o, in0=a, scalar=sc, in1=b, op0=op0, op1=op1, **kw)

        def f_rec(o, i):
            return lambda e: e.reciprocal(out=o, in_=i)

        def f_ms(o, v):
            return lambda e: e.memset(o, v)

        def copy_op(eng, out, in_, reads, writes, scale=None):
            if eng == "act":
                if scale is None:
                    S.op("act", lambda e: e.copy(out=out, in_=in_), reads=reads, writes=writes)
                else:
                    S.op("act", lambda e: e.mul(out=out, in_=in_, mul=scale), reads=reads, writes=writes)
            else:
                if scale is None:
                    S.op("dve", lambda e: e.tensor_copy(out=out, in_=in_), reads=reads, writes=writes)
                else:
                    S.op("dve", lambda e: e.tensor_scalar(out=out, in0=in_, scalar1=scale, scalar2=None, op0=ALU.mult), reads=reads, writes=writes)

        S.op("pool", lambda e: e.memset(identf[:], 0.0), writes=[r_const])
        S.op("pool", lambda e: e.affine_select(out=identf[:], in_=identf[:], pattern=[[-1, 128]], compare_op=ALU.not_equal,
                                               fill=1.0, base=0, channel_multiplier=1), reads=[r_const], writes=[r_const])
        S.op("pool", lambda e: e.memset(epsb[:], 1e-6), writes=[r_const])
        S.op("dve", lambda e: e.tensor_copy(out=ident[:], in_=identf[:]), reads=[r_const], writes=[r_const])
        r_sc = Res("sc")
        cv = sbt("cv", [2, D], F32)
        cvs = sbt("cvs", [2, D], F32)
        S.dma("sp", cv[:], cvec[:, :], writes=[r_sc])
        S.op("act", f_act(cvs[:], cv[:], AF.Silu), reads=[r_sc], writes=[r_sc])
        pb0, r_pb0 = psf[0]
        S.op("pe", [(lambda o, i: (lambda e: e.transpose(o, i, identf[0:2, 0:2])))(pb0[:, k * 2:(k + 1) * 2], cvs[0:2, k * 128:(k + 1) * 128]) for k in range(8)],
             reads=[r_sc, r_const], writes=[r_pb0])
        S.op("dve", lambda e: e.tensor_copy(out=scT[:], in_=pb0[:, 0:16].rearrange("p (k r) -> p k r", r=2)), reads=[r_pb0], writes=[r_const])

        r_xa = Res("xa", multi=True)
        r_xb = Res("xb", multi=True)
        r_xloc = Res("xloc", multi=True)
        r_XTloc = Res("XTloc", multi=True)
        for i in range(8):
            S.dma("sp", xb[PADR + i * 1024:PADR + (i + 1) * 1024, :], x_in[i * 1024:(i + 1) * 1024, :], writes=[r_xb])
        S.dma("sp", xb[CT0:CT0 + TC, :], ctx_in[:, :], writes=[r_xb])
        zt = sbt("zt", [128, D], F32)
        r_zt = Res("zt")
        S.op("pool", f_ms(zt[:], 0.0), writes=[r_zt])
        for i in range(4):
            S.dma("sp", xb[i * 128:(i + 1) * 128, :], zt[:], reads=[r_zt], writes=[r_xb])
            S.dma("sp", xb[PADR + T + i * 128:PADR + T + (i + 1) * 128, :], zt[:], reads=[r_zt], writes=[r_xb])

        _offc = {}

        def offv(e):
            if "v" not in _offc:
                _offc["v"] = e.snap((e.partition_id() % 2) * HALF, min_val=0, max_val=HALF)
            return _offc["v"]

        def dyn_rows(dst, src, row0, nrows):
            sl = src[row0:row0 + HALF + nrows, :]
            return lambda e: e.dma_start(out=dst, in_=sl[bass.ds(offv(e), nrows), :])

        def dyn_cols(dst, src, col0, ncols, pat, **kw):
            sl = src[:, col0:col0 + HALF + ncols]
            return lambda e: e.dma_start(out=dst, in_=sl[:, bass.ds(offv(e), ncols)].rearrange(pat, **kw))
        r_fw = Res("fw", multi=True)
        r_mw = Res("mw", multi=True)

        def bg_casts():
            for b_ in range(11):
                S.dma("pool", fwg[b_].rearrange("p (k f) -> p k f", k=8), ffn_wg[0, :, b_ * 256:(b_ + 1) * 256].rearrange("(k p) f -> p k f", p=128), writes=[r_fw])
                S.dma("pool", fwu[b_].rearrange("p (k f) -> p k f", k=8), ffn_wu[0, :, b_ * 256:(b_ + 1) * 256].rearrange("(k p) f -> p k f", p=128), writes=[r_fw])
            for k in range(11):
                S.dma("pool", fwd[k * 256:(k + 1) * 256, :], ffn_wd[0, k * 256:(k + 1) * 256, :], writes=[r_fw])
            for e_ in range(8):
                for k in range(2):
                    for q in range(4):
                        r0 = e_ * D + k * 512
                        S.dma("pool", mwgu[q][r0:r0 + 512, 0:896], moe_wg[0, e_, k * 512:(k + 1) * 512, q * 896:(q + 1) * 896], writes=[r_mw])
                        S.dma("pool", mwgu[q][r0:r0 + 512, 896:1792], moe_wu[0, e_, k * 512:(k + 1) * 512, q * 896:(q + 1) * 896], writes=[r_mw])
                for k in range(14):
                    S.dma("pool", mwd[e_, k * 256:(k + 1) * 256, :], moe_wd[0, e_, k * 256:(k + 1) * 256, :], writes=[r_mw])

        R = {n: Res(n, multi=True) for n in ("U", "qT", "kT", "v", "pT", "bT", "gT", "Gd", "XT", "attnT", "convT", "h2T", "modd", "gates")}

        SEQS = [(0, T, 0), (T, TC, 1)]

        def norm_tile(xt, r_x, gm, sh, r_mod, out_bf, r_out, tmp, r_tmp, ss, r_ss, out_f32=None):
            S.op("act", f_act(tmp, xt, AF.Square, accum_out=ss), reads=[r_x], writes=[r_tmp, r_ss])
            S.op("act", f_act(ss, ss, AF.Sqrt, bias=epsb[:], scale=1.0 / D), reads=[r_ss, r_const], writes=[r_ss])
            S.op("dve", f_rec(ss, ss), reads=[r_ss], writes=[r_ss])
            S.op("dve", f_stt(tmp, xt, ss, gm, ALU.mult, ALU.mult), reads=[r_x, r_ss, r_mod], writes=[r_tmp])
            if out_f32 is None:
                S.op("pool", f_tt(out_bf, tmp, sh, ALU.add), reads=[r_tmp, r_mod], writes=[r_out])
            else:
                S.op("pool", f_tt(out_f32, tmp, sh, ALU.add), reads=[r_tmp, r_mod], writes=[r_out])
                S.op("act", (lambda o, i: (lambda e: e.copy(out=o, in_=i)))(out_bf, out_f32), reads=[r_out], writes=[r_out])

        def transpose_to(hb, r_hb, dst, r_dst, nchunk):
            pt, r_pt = PB.next()
            S.op("pe", [f_tr(pt[:, k * 128:(k + 1) * 128], hb[:, k * 128:(k + 1) * 128]) for k in range(nchunk)],
                 reads=[r_hb, r_const], writes=[r_pt])
            copy_op(evac_eng(), dst, pt[:, 0:nchunk * 128].rearrange("p (k t) -> p k t", t=128), [r_pt], [r_dst])

        try:
          for l in range(2):
            last = (l == 1)
            if stop == (l, "P0"):
                raise _StopBuild()
            S.barrier(); AR.reset()
            wada = AR.alloc([8, 6 * D], BF16)
            r_w = Res("wada", multi=True)
            for k in range(8):
                S.dma("pool", wada[:, k, :], w_ada[l, k * 128:(k + 1) * 128, :], writes=[r_w])
            bada = AR.alloc([6 * D], F32, parts=2)
            r_b = Res("bada")
            S.dma("sp", bada, b_ada[l:l + 1, :].partition_broadcast(2), writes=[r_b])
            modt = AR.alloc([6 * D], F32, parts=2)
            r_mt = Res("modt", multi=True)
            for j in range(12):
                pb, r_pb = PF.next()
                S.op("pe", [f_mm(pb[0:2, :], scT[:, k, :], wada[:, k, j * 512:(j + 1) * 512], k == 0, k == 7) for k in range(8)],
                     reads=[r_w, r_const], writes=[r_pb])
                S.op("dve", f_tt(modt[:, j * 512:(j + 1) * 512], pb[0:2, :], bada[:, j * 512:(j + 1) * 512], ALU.add),
                     reads=[r_pb, r_b], writes=[r_mt])
            S.dma("sp", modd[:, :], modt, reads=[r_mt], writes=[R["modd"]])

            if stop == (l, "P1"):
                raise _StopBuild()
            S.barrier(); AR.reset()
            win = AR.alloc([8, PW], BF16)
            r_win = Res("win", multi=True)
            for k in range(8):
                S.dma("pool", win[:, k, :], w_in[l, k * 128:(k + 1) * 128, :], writes=[r_win])
            if l == 0:
                bg_casts()
            g1 = AR.alloc([D], F32)
            r_g = Res("g1")
            S.dma("sp", g1, norm1_g[l:l + 1, :].partition_broadcast(128), writes=[r_g])
            gms, shs, r_mods = [], [], []
            for (t0, tl, row) in SEQS:
                gm = AR.alloc([D], F32); sh = AR.alloc([D], F32); r_m = Res("mod%d" % row, multi=True)
                S.dma("sp", sh, modd[row:row + 1, 0:D].partition_broadcast(128), reads=[R["modd"]], writes=[r_m])
                S.dma("sp", gm, modd[row:row + 1, D:2 * D].partition_broadcast(128), reads=[R["modd"]], writes=[r_m])
                S.op("dve", f_stt(gm, gm, 1.0, g1, ALU.add, ALU.mult), reads=[r_m, r_g], writes=[r_m])
                gms.append(gm); shs.append(sh); r_mods.append(r_m)
            xts = Ring([(AR.alloc([D], F32), Res("xt%d" % i)) for i in range(2)])
            tmps = Ring([(AR.alloc([D], F32), Res("tmpf%d" % i)) for i in range(2)])
            sss = Ring([(AR.alloc([1], F32), Res("ss%d" % i)) for i in range(2)])
            hbs = Ring([(AR.alloc([D], BF16), Res("hb%d" % i)) for i in range(2)])
            hTs = Ring([(AR.alloc([8, 512], BF16), Res("hT%d" % i, multi=True)) for i in range(2)])
            stg = Ring([(AR.alloc([512], BF16), Res("stg%d" % i)) for i in range(6)])
            ustg = Ring([(AR.alloc([2, 512], BF16), Res("ustg%d" % i, multi=True)) for i in range(2)])
            vstg = Ring([(AR.alloc([8, 65], BF16), Res("vstg%d" % i)) for i in range(2)])
            for (vt, r_vt) in vstg.items:
                S.op("pool", f_ms(vt, 1.0), writes=[r_vt])
            def p1_group(xload, gs, si, col0, do_u, do_rest):
                hT, r_hT = hTs.next()
                for tt in range(gs // 128):
                    xt, r_xt = xts.next()
                    xload(tt, xt, r_xt)
                    hb, r_hb = hbs.next()
                    tmpf, r_tmpf = tmps.next(); ssq, r_ssq = sss.next()
                    norm_tile(xt, r_xt, gms[si], shs[si], r_mods[si], hb, r_hb, tmpf, r_tmpf, ssq, r_ssq)
                    transpose_to(hb, r_hb, hT[:, :, tt * 128:(tt + 1) * 128], r_hT, 8)
                g0 = col0
                if do_rest:
                    ust, r_ust = ustg.next()
                    for ch in range(44):
                        c0 = ch * 128
                        if ch in (0, 1, 10, 11, 12, 13):
                            continue
                        pb, r_pb = PF.next()
                        S.op("pe", [f_mm(pb[:, 0:gs], win[:, k, c0:c0 + 128], hT[:, k, 0:gs], k == 0, k == 7) for k in range(8)],
                             reads=[r_win, r_hT], writes=[r_pb])
                        if 14 <= ch <= 15:
                            copy_op(evac_eng(), ust[:, ch - 14, 0:gs], pb[:, 0:gs], [r_pb], [r_ust])
                            continue
                        st, r_st = stg.next()
                        if 2 <= ch <= 5:
                            copy_op(evac_eng(), st[:, 0:gs], pb[:, 0:gs], [r_pb], [r_st], scale=0.125)
                            dst, rd = qTd[(ch - 2) * 128:(ch - 1) * 128, g0:g0 + gs], R["qT"]
                        elif 6 <= ch <= 9:
                            copy_op(evac_eng(), st[:, 0:gs], pb[:, 0:gs], [r_pb], [r_st])
                            dst, rd = kTd[(ch - 6) * 128:(ch - 5) * 128, g0:g0 + gs], R["kT"]
                        elif 16 <= ch <= 17:
                            copy_op(evac_eng(), st[:, 0:gs], pb[:, 0:gs], [r_pb], [r_st])
                            dst, rd = bTd[(ch - 16) * 128:(ch - 15) * 128, g0:g0 + gs], R["bT"]
                        elif 18 <= ch <= 19:
                            S.op("dve", f_tt(st[:, 0:gs], pb[:, 0:gs], ust[:, ch - 18, 0:gs], ALU.mult), reads=[r_pb, r_ust], writes=[r_st])
                            dst, rd = pTd[(ch - 18) * 128:(ch - 17) * 128, g0:g0 + gs], R["pT"]
                        else:
                            S.op("act", f_act(st[:, 0:gs], pb[:, 0:gs], AF.Sigmoid), reads=[r_pb], writes=[r_st])
                            dst, rd = gTd[(ch - 20) * 128:(ch - 19) * 128, g0:g0 + gs], R["gT"]
                        S.dma("sp", dst, st[:, 0:gs], reads=[r_st], writes=[rd])
                for tt in range(gs // 128):
                    tok0 = g0 + tt * 128
                    if do_u:
                        pb, r_pb = PF.next()
                        S.op("pe", [f_mm(pb[:, 0:256], hT[:, k, tt * 128:(tt + 1) * 128], win[:, k, 0:256], k == 0, k == 7) for k in range(8)],
                             reads=[r_win, r_hT], writes=[r_pb])
                        st, r_st = stg.next()
                        copy_op(evac_eng(), st[:, 0:256], pb[:, 0:256], [r_pb], [r_st])
                        S.dma("sp", Ud[tok0:tok0 + 128, :], st[:, 0:256], reads=[r_st], writes=[R["U"]])
                    if do_rest:
                        pb, r_pb = PF.next()
                        S.op("pe", [f_mm(pb[:, :], hT[:, k, tt * 128:(tt + 1) * 128], win[:, k, 1280:1792], k == 0, k == 7) for k in range(8)],
                             reads=[r_win, r_hT], writes=[r_pb])
                        vt, r_vt = vstg.next()
                        copy_op(evac_eng(), vt[:, :, 0:64], pb[:, :].rearrange("p (h d) -> p h d", d=64), [r_pb], [r_vt])
                        S.dma("sp", vd[tok0:tok0 + 128, :], vt.rearrange("p h d -> p (h d)"), reads=[r_vt], writes=[R["v"]])

            def static_xload(g0):
                def f(tt, xt, r_xt):
                    r0 = xbrow(g0 + tt * 128)
                    S.dma("sp", xt, xb[r0:r0 + 128, :], reads=[r_xb], writes=[r_xt])
                return f

            def dyn_xload(row0):
                def f(tt, xt, r_xt):
                    S.dma("sp", xt, xloc[row0 + tt * 128:row0 + (tt + 1) * 128, :], reads=[r_xloc], writes=[r_xt])
                return f

            if last:
                for i5 in range(LT // 1024):
                    S.dma_fn("sp", dyn_rows(xloc[i5 * 1024:(i5 + 1) * 1024, :], xb, i5 * 1024, 1024), reads=[r_xb], writes=[r_xloc])
            if not last:
                for g0 in range(0, T, 512):
                    p1_group(static_xload(g0), 512, 0, g0, True, True)
                p1_group(static_xload(T), TC, 1, T, True, True)
            else:
                for g0 in range(0, T, 512):
                    p1_group(static_xload(g0), 512, 0, g0, True, False)
                p1_group(static_xload(T), TC, 1, T, False, True)
                for j in range(LT // 512):
                    p1_group(dyn_xload(j * 512), 512, 0, j * 512, False, True)

            if stop == (l, "P2"):
                raise _StopBuild()
            S.barrier(); AR.reset()
            t64 = AR.alloc([3, 64], BF16, parts=64); r_t64 = Res("t64")
            S.dma("sp", t64, dft64.rearrange("p (a b) -> p a b", b=64), writes=[r_t64])
            ubs = Ring([(AR.alloc([8, 256], BF16), Res("ub%d" % i)) for i in range(3)])
            tbs = Ring([(AR.alloc([8, 2, 128], BF16), Res("tb%d" % i)) for i in range(3)])
            gss = Ring([(AR.alloc([8, 512], BF16), Res("gs%d" % i, multi=True)) for i in range(3)])
            Uv = Ud[0:T, :].rearrange("(a b) c -> a b c", b=64)
            dAv = dftA.rearrange("p (a r k) -> p a r k", r=2, k=128)
            for lb in range(8):
                ub, r_ub = ubs.next(); tb, r_tb = tbs.next(); gsb, r_gs = gss.next()
                S.dma("sp", ub, Uv[:, lb * 8:(lb + 1) * 8, :], reads=[R["U"]], writes=[r_ub])
                S.dma("sp", tb, dAv[:, lb * 8:(lb + 1) * 8, :, :], writes=[r_tb])
                for i in range(8):
                    pb, r_pb = PF.next()
                    S.op("pe", [f_mm(pb[:, pr * 256:(pr + 1) * 256], tb[:, i, pr, :], ub[:, i, :], True, True) for pr in range(2)],
                         reads=[r_ub, r_tb], writes=[r_pb])
                    copy_op(evac_eng(), gsb[:, i, :], pb[:, :], [r_pb], [r_gs])
                S.dma("sp", Gd[lb * 8:(lb + 1) * 8, :, :].rearrange("l k c -> k l c"), gsb, reads=[r_gs], writes=[R["Gd"]])
            xts4 = [(AR.alloc([64, 128], BF16), Res("xts%d" % i, multi=True)) for i in range(4)]
            gbs = Ring([(AR.alloc([8, 512], BF16, parts=64), Res("gb%d" % i)) for i in range(3)])
            for kb in range(16):
                gb, r_gb = gbs.next()
                S.dma("sp", gb, Gd[:, kb * 8:(kb + 1) * 8, :], reads=[R["Gd"]], writes=[r_gb])
                for ri in range(2):
                    for chalf in range(2):
                        pb, r_pb = PF.next()
                        fns = []
                        c0 = chalf * 128
                        for i in range(8):
                            o = pb[:, i * 64:(i + 1) * 64]
                            if ri == 0:
                                fns.append(f_mm(o, gb[:, i, c0:c0 + 128], t64[:, 0, :], True, False))
                                fns.append(f_mm(o, gb[:, i, 256 + c0:256 + c0 + 128], t64[:, 1, :], False, True))
                            else:
                                fns.append(f_mm(o, gb[:, i, 256 + c0:256 + c0 + 128], t64[:, 0, :], True, False))
                                fns.append(f_mm(o, gb[:, i, c0:c0 + 128], t64[:, 2, :], False, True))
                        S.op("pe", fns, reads=[r_gb, r_t64], writes=[r_pb])
                        xt4, r_x4 = xts4[ri * 2 + chalf]
                        copy_op(evac_eng(), xt4[:, :, kb * 8:(kb + 1) * 8].rearrange("p a b -> p b a"), pb[:, :].rearrange("p (b a) -> p b a", a=64), [r_pb], [r_x4])
            for i4 in range(4):
                xt4, r_x4 = xts4[i4]
                S.dma("sp", XTd[i4 * 128:(i4 + 1) * 128, 0:T], xt4.rearrange("p a b -> p (a b)"), reads=[r_x4], writes=[R["XT"]])
            uc = AR.alloc([2, 256], BF16); r_uc = Res("uc")
            S.dma("sp", uc, Ud[T:TT, :].rearrange("(a p) c -> p a c", p=128), reads=[R["U"]], writes=[r_uc])
            tcx = AR.alloc([2, 2, 256], BF16); r_tc = Res("tcx")
            S.dma("sp", tcx, dftc.rearrange("(a p) (r k) -> p a r k", p=128, r=2), writes=[r_tc])
            xcs = AR.alloc([4, 256], BF16); r_xc = Res("xcs", multi=True)
            for ri in range(2):
                for chalf in range(2):
                    pb, r_pb = PF.next()
                    S.op("pe", [f_mm(pb[:, 0:256], uc[:, a, chalf * 128:(chalf + 1) * 128], tcx[:, a, ri, :], a == 0, a == 1) for a in range(2)],
                         reads=[r_uc, r_tc], writes=[r_pb])
                    copy_op(evac_eng(), xcs[:, ri * 2 + chalf, :], pb[:, 0:256], [r_pb], [r_xc])
            S.dma("sp", XTd[:, T:TT].rearrange("(j p) t -> p j t", p=128), xcs, reads=[r_xc], writes=[R["XT"]])
            if last:
                for i4 in range(4):
                    S.dma_fn("sp", (lambda d_, sl_: (lambda e: e.dma_start(out=d_, in_=sl_[:, bass.ds(offv(e), HALF)])))(XTloc[i4 * 128:(i4 + 1) * 128, :], XTd[i4 * 128:(i4 + 1) * 128, 0:T]),
                             reads=[R["XT"]], writes=[r_XTloc])

            if stop == (l, "P3"):
                raise _StopBuild()
            S.barrier(); AR.reset()
            NW = 6 if last else 5
            bias = AR.alloc([5, 8, NW, 128], BF16); r_bias = Res("bias", multi=True)
            bsrc = biasT1 if last else biasT
            for v_ in range(5):
                S.dma("sp", bias[:, v_], bsrc[v_].rearrange("p (h j q) -> p h j q", h=8, j=NW), writes=[r_bias])
            for v_ in range(5):
                S.op("act", f_act(bias[:, v_].rearrange("p h j q -> p (h j q)"), bias[:, v_].rearrange("p h j q -> p (h j q)"), AF.Exp), reads=[r_bias], writes=[r_bias])
            Kc = AR.alloc([4, 256], BF16); Vc = AR.alloc([2, 520], BF16); r_kvc = Res("kvc", multi=True)
            S.dma("sp", Kc, kTd[:, T:TT].rearrange("(j p) t -> p j t", p=128), reads=[R["kT"]], writes=[r_kvc])
            S.dma("sp", Vc, vd[T:TT, :].rearrange("(j p) c -> p j c", p=128), reads=[R["v"]], writes=[r_kvc])
            Qs = Ring([(AR.alloc([4, 512], BF16), Res("Q%d" % i)) for i in range(2)])
            Kws = Ring([(AR.alloc([4, NW * 128], BF16), Res("Kw%d" % i)) for i in range(3)])
            Vws = Ring([(AR.alloc([NW, 520], BF16), Res("Vw%d" % i)) for i in range(3)])
            pTs = Ring([(AR.alloc([(NW + 2) * 128], BF16), Res("pT%d" % i, multi=True)) for i in range(4)])
            recs = Ring([(AR.alloc([8], F32), Res("rec%d" % i, multi=True)) for i in range(2)])
            atts = Ring([(AR.alloc([8, 64], BF16), Res("att%d" % i, multi=True)) for i in range(2)])
            aTst = Ring([(AR.alloc([4, 512], BF16), Res("aTst%d" % i, multi=True)) for i in range(2)])
            PS4 = Ring(psf[0:4])
            if not last:
                tiles = [(128 * m, 64 * min(max(2 * m - 4, 0), 118), {0: 1, 1: 2, 62: 3, 63: 4}.get(m, 0), True) for m in range(64)]
                tiles += [(T, 0, 0, False), (T + 128, 0, 0, False)]
            else:
                tiles = [(PADR + 128 * m, PADR + 128 * m - (384 if m == 31 else 256), {0: 1, 1: 2, 30: 3, 31: 4}.get(m, 0), True) for m in range(32)]
            pO = [psf[4], psf[5]]
            tstate = {"Q": None, "r_Q": None, "aT": None, "r_aT": None}

            def att_pre(ti):
                tok0, kcol, var, lat = tiles[ti]
                c = {"tok0": tok0, "var": var, "lat": lat, "ti": ti}
                if lat:
                    if ti % 4 == 0:
                        tstate["Q"], tstate["r_Q"] = Qs.next()
                        S.dma("sp", tstate["Q"], qTd[:, tok0:tok0 + 512].rearrange("(j p) t -> p j t", p=128), reads=[R["qT"]], writes=[tstate["r_Q"]])
                        tstate["aT"], tstate["r_aT"] = aTst.next()
                    c["qo"] = (ti % 4) * 128
                    c["Kw"], c["r_Kw"] = Kws.next(); c["Vw"], c["r_Vw"] = Vws.next()
                    S.dma("sp", c["Kw"], kTd[:, kcol:kcol + NW * 128].rearrange("(j p) t -> p j t", p=128), reads=[R["kT"]], writes=[c["r_Kw"]])
                    S.dma("sp", c["Vw"], vd[kcol:kcol + NW * 128, :].rearrange("(j p) c -> p j c", p=128), reads=[R["v"]], writes=[c["r_Vw"]])
                    c["nw"] = NW
                else:
                    if tok0 == T:
                        tstate["Q"], tstate["r_Q"] = Qs.next()
                        S.dma("sp", tstate["Q"][:, :, 0:256], qTd[:, T:TT].rearrange("(j p) t -> p j t", p=128), reads=[R["qT"]], writes=[tstate["r_Q"]])
                        tstate["aT"], tstate["r_aT"] = aTst.next()
                    c["qo"] = tok0 - T
                    c["nw"] = 0
                c["Q"], c["r_Q"], c["aT"], c["r_aT"] = tstate["Q"], tstate["r_Q"], tstate["aT"], tstate["r_aT"]
                c["nk"] = c["nw"] + 2
                return c

            def att_S(c, h):
                nw, nk, lat, Q, qo = c["nw"], c["nk"], c["lat"], c["Q"], c["qo"]
                hp, po = h // 2, (h % 2) * 64
                nbk = (nk + 3) // 4
                pS = [PS4.next() for _ in range(nbk)]
                fns = []
                for j in range(nk):
                    o = pS[j // 4][0][:, (j % 4) * 128:(j % 4 + 1) * 128]
                    if j < nw:
                        fns.append(f_mm(o, c["Kw"][po:po + 64, hp, j * 128:(j + 1) * 128], Q[po:po + 64, hp, qo:qo + 128], True, True))
                    else:
                        jj = j - nw
                        fns.append(f_mm(o, Kc[po:po + 64, hp, jj * 128:(jj + 1) * 128], Q[po:po + 64, hp, qo:qo + 128], True, True))
                S.op("pe", fns, reads=[c["r_Q"], r_kvc] + ([c["r_Kw"]] if lat else []), writes=[p_[1] for p_ in pS])
                pT, r_pT = pTs.next()
                for bk in range(nbk):
                    n_ = min(4, nk - 4 * bk) * 128
                    S.op("act", f_act(pT[:, bk * 512:bk * 512 + n_], pS[bk][0][:, 0:n_], AF.Exp), reads=[pS[bk][1]], writes=[r_pT])
                if lat:
                    S.op("dve", f_tt(pT[:, 0:nw * 128], pT[:, 0:nw * 128], bias[:, c["var"], h].rearrange("p j q -> p (j q)"), ALU.mult), reads=[r_pT, r_bias], writes=[r_pT])
                return (pT, r_pT)

            def att_PV(c, h, pTt):
                pT, r_pT = pTt
                nw, nk, lat = c["nw"], c["nk"], c["lat"]
                po_t, r_po = pO[h // 4]
                o = po_t[:, (h % 4) * 65:(h % 4 + 1) * 65]
                fns = []
                for j in range(nk):
                    if j < nw:
                        fns.append(f_mm(o, pT[:, j * 128:(j + 1) * 128], c["Vw"][:, j, h * 65:(h + 1) * 65], j == 0, False))
                    else:
                        jj = j - nw
                        fns.append(f_mm(o, pT[:, j * 128:(j + 1) * 128], Vc[:, jj, h * 65:(h + 1) * 65], j == 0, j == nk - 1))
                S.op("pe", fns, reads=[r_pT, r_kvc] + ([c["r_Vw"]] if lat else []), writes=[r_po])

            def att_norm(c):
                rec, r_rec = recs.next(); att, r_att = atts.next()
                for hh in range(2):
                    po_t, r_po = pO[hh]
                    pv = po_t[:, 0:260].rearrange("p (h d) -> p h d", d=65)
                    S.op("dve", f_rec(rec[:, hh * 4:(hh + 1) * 4], pv[:, :, 64]), reads=[r_po], writes=[r_rec])
                    S.op("dve", f_tt(att[:, hh * 4:(hh + 1) * 4, :], pv[:, :, 0:64], rec[:, hh * 4:(hh + 1) * 4].unsqueeze(2).to_broadcast([128, 4, 64]), ALU.mult),
                         reads=[r_po, r_rec], writes=[r_att])
                c["att"], c["r_att"] = att, r_att

            def att_post(c):
                tok0, lat, qo, aT, r_aT = c["tok0"], c["lat"], c["qo"], c["aT"], c["r_aT"]
                transpose_to(c["att"].rearrange("p h d -> p (h d)"), c["r_att"], aT[:, :, qo:qo + 128], r_aT, 4)
                if lat and c["ti"] % 4 == 3:
                    S.dma("sp", attnTd[:, tok0 - 384:tok0 + 128].rearrange("(j p) t -> p j t", p=128), aT, reads=[r_aT], writes=[R["attnT"]])
                elif (not lat) and tok0 == T + 128:
                    S.dma("sp", attnTd[:, T:TT].rearrange("(j p) t -> p j t", p=128), aT[:, :, 0:256], reads=[r_aT], writes=[R["attnT"]])

            items = [(ti, h) for ti in range(len(tiles)) for h in range(8)]
            ctxs = {0: att_pre(0)}
            pts = {0: att_S(ctxs[0], 0)}
            pending = None
            for i, (ti, h) in enumerate(items):
                if h == 2 and ti + 1 < len(tiles):
                    ctxs[ti + 1] = att_pre(ti + 1)
                if i + 1 < len(items):
                    nti, nh_ = items[i + 1]
                    if nh_ == 0 and nti not in ctxs:
                        ctxs[nti] = att_pre(nti)
                    pts[i + 1] = att_S(ctxs[nti], nh_)
                if pending is not None:
                    att_post(pending)
                    pending = None
                att_PV(ctxs[ti], h, pts.pop(i))
                if h == 7:
                    att_norm(ctxs[ti])
                    pending = ctxs.pop(ti)
            if pending is not None:
                att_post(pending)

            if stop == (l, "P4"):
                raise _StopBuild()
            S.barrier(); AR.reset()
            cw = AR.alloc([2, 3], F32); r_cw = Res("cw")
            cw3 = AR.alloc([256], F32, parts=3); r_cw3 = Res("cw3")
            S.dma("sp", cw3, conv_w[l], writes=[r_cw3])
            pbc, r_pbc = PF.next()
            S.op("pe", [(lambda o, i: (lambda e: e.transpose(o, i, identf[0:3, 0:3])))(pbc[:, c_ * 3:(c_ + 1) * 3], cw3[0:3, c_ * 128:(c_ + 1) * 128]) for c_ in range(2)],
                 reads=[r_cw3, r_const], writes=[r_pbc])
            S.op("dve", (lambda o, i: (lambda e: e.tensor_copy(out=o, in_=i)))(cw, pbc[:, 0:6].rearrange("p (c j) -> p c j", j=3)), reads=[r_pbc], writes=[r_cw])
            pins = Ring([(AR.alloc([2, 514], BF16), Res("pin%d" % i, multi=True)) for i in range(2)])
            bins = Ring([(AR.alloc([2, 512], BF16), Res("bin%d" % i)) for i in range(2)])
            cts = Ring([(AR.alloc([512], F32), Res("ct%d" % i)) for i in range(2)])
            cos_ = Ring([(AR.alloc([2, 512], BF16), Res("co%d" % i, multi=True)) for i in range(2)])
            if not last:
                cgroups = [(g0, 512, 0, T) for g0 in range(0, T, 512)] + [(T, TC, T, TT)]
            else:
                cgroups = [(512 * j, 512, 0, LT) for j in range(1, 9)]
                emk = AR.alloc([2], F32); r_emk = Res("emk")
                S.dma("sp", emk, edgemask[0:1, :].partition_broadcast(128), writes=[r_emk])
            for (g0, gs, t0, tend) in cgroups:
                if True:
                    tl = tend - t0
                    pin, r_pin = pins.next(); bi, r_bi = bins.next(); co, r_co = cos_.next()
                    lo = g0 - 1 if g0 > t0 else g0
                    hi = g0 + gs + 1 if g0 + gs < t0 + tl else g0 + gs
                    if lo == g0:
                        S.op("pool", f_ms(pin[:, :, 0:1], 0.0), writes=[r_pin])
                    if hi == g0 + gs:
                        S.op("pool", f_ms(pin[:, :, gs + 1:gs + 2], 0.0), writes=[r_pin])
                    S.dma("sp", pin[:, :, lo - g0 + 1:hi - g0 + 1], pTd[:, lo:hi].rearrange("(c p) t -> p c t", p=128), reads=[R["pT"]], writes=[r_pin])
                    S.dma("sp", bi[:, :, 0:gs], bTd[:, g0:g0 + gs].rearrange("(c p) t -> p c t", p=128), reads=[R["bT"]], writes=[r_bi])
                    if last and g0 == 512:
                        S.op("dve", f_ts(pin[:, :, 0:1], pin[:, :, 0:1], emk[:, 0:1], ALU.mult), reads=[r_pin, r_emk], writes=[r_pin])
                    if last and g0 == 512 * 8:
                        S.op("dve", f_ts(pin[:, :, gs + 1:gs + 2], pin[:, :, gs + 1:gs + 2], emk[:, 1:2], ALU.mult), reads=[r_pin, r_emk], writes=[r_pin])
                    for c in range(2):
                        ct, r_ct = cts.next()
                        S.op("dve", f_ts(ct[:, 0:gs], pin[:, c, 1:gs + 1], cw[:, c, 1:2], ALU.mult), reads=[r_pin, r_cw], writes=[r_ct])
                        S.op("dve", f_stt(ct[:, 0:gs], pin[:, c, 0:gs], cw[:, c, 0:1], ct[:, 0:gs], ALU.mult, ALU.add), reads=[r_pin, r_cw, r_ct], writes=[r_ct])
                        S.op("dve", f_stt(ct[:, 0:gs], pin[:, c, 2:gs + 2], cw[:, c, 2:3], ct[:, 0:gs], ALU.mult, ALU.add), reads=[r_pin, r_cw, r_ct], writes=[r_ct])
                        S.op("pool", f_tt(co[:, c, 0:gs], ct[:, 0:gs], bi[:, c, 0:gs], ALU.mult), reads=[r_ct, r_bi], writes=[r_co])
                    S.dma("sp", convTd[:, g0:g0 + gs].rearrange("(c p) t -> p c t", p=128), co[:, :, 0:gs], reads=[r_co], writes=[R["convT"]])

            if stop == (l, "P5a"):
                raise _StopBuild()
            S.barrier(); AR.reset()
            seqs5 = SEQS if not last else SEQS[:1]
            wf = AR.alloc([2, D], BF16); cbd = AR.alloc([2, 2, 256], BF16); r_wf = Res("wf", multi=True)
            S.dma("pool", wf, w_fourier[l].rearrange("(k p) n -> p k n", p=128), writes=[r_wf])
            S.dma("sp", cbd, c64bd.rearrange("(k p) (r c) -> p k r c", p=128, r=2), writes=[r_wf])
            wcs = AR.alloc([4, D], BF16); r_wcs = Res("wcs", multi=True)
            wna = AR.alloc([4, D], BF16); wco = AR.alloc([2, D], BF16); wo = AR.alloc([8, D], BF16); r_wm = Res("wm", multi=True)
            S.dma("pool", wna, w_na[l].rearrange("(k p) n -> p k n", p=128), writes=[r_wm])
            S.dma("pool", wco, w_conv_out[l].rearrange("(k p) n -> p k n", p=128), writes=[r_wm])
            S.dma("pool", wo, w_out[l].rearrange("(k p) n -> p k n", p=128), writes=[r_wm])
            for part in range(2):
                for chalf in range(2):
                    for nh in range(2):
                        pb, r_pb = PF.next()
                        S.op("pe", [f_mm(pb[:, :], cbd[:, k, part, chalf * 128:(chalf + 1) * 128], wf[:, k, nh * 512:(nh + 1) * 512], k == 0, k == 1) for k in range(2)],
                             reads=[r_wf], writes=[r_pb])
                        copy_op(evac_eng(), wcs[:, part * 2 + chalf, nh * 512:(nh + 1) * 512], pb[:, :], [r_pb], [r_wcs])
            g2 = AR.alloc([D], F32); r_g2 = Res("g2")
            S.dma("sp", g2, norm2_g[l:l + 1, :].partition_broadcast(128), writes=[r_g2])
            m5 = []
            for (t0, tl, row) in seqs5:
                gt = AR.alloc([D], F32); gm = AR.alloc([D], F32); sh = AR.alloc([D], F32); r_m = Res("m5_%d" % row, multi=True)
                S.dma("sp", gt, modd[row:row + 1, 2 * D:3 * D].partition_broadcast(128), reads=[R["modd"]], writes=[r_m])
                S.dma("sp", sh, modd[row:row + 1, 3 * D:4 * D].partition_broadcast(128), reads=[R["modd"]], writes=[r_m])
                S.dma("sp", gm, modd[row:row + 1, 4 * D:5 * D].partition_broadcast(128), reads=[R["modd"]], writes=[r_m])
                S.op("dve", f_stt(gm, gm, 1.0, g2, ALU.add, ALU.mult), reads=[r_m, r_g2], writes=[r_m])
                m5.append((gt, gm, sh, r_m))
            if last:
                wrb = AR.alloc([8, D], F32); r_wrb = Res("wrb", multi=True)
                wr_ = wrb[:, 0, 0:64].rearrange("p (k e) -> p k e", e=8); r_wr = Res("wr")
                wrTs = wrb[0:8, 1, :]; r_wrTs = Res("wrTs", multi=True)
                S.dma("sp", wr_, moe_router[0].rearrange("(p k) e -> p k e", k=8), writes=[r_wr])
                for hf_ in range(2):
                    pbr, r_pbr = PF.next()
                    S.op("pe", [(lambda o, i: (lambda e: e.transpose(o, i, identf[:, :])))(pbr[0:8, kk * 128:(kk + 1) * 128], wr_[:, hf_ * 4 + kk, :]) for kk in range(4)],
                         reads=[r_wr, r_const], writes=[r_pbr])
                    S.op("dve", (lambda o, i: (lambda e: e.tensor_copy(out=o, in_=i)))(
                        wrTs.rearrange("e (p k) -> e k p", k=8)[:, hf_ * 4:(hf_ + 1) * 4, :], pbr[0:8, :].rearrange("e (k p) -> e k p", p=128)),
                        reads=[r_pbr], writes=[r_wrTs])
                r_wrTd = Res("wrTd")
                S.dma("sp", wrT[:, :], wrTs, reads=[r_wrTs], writes=[r_wrTd])
                for e_ in range(8):
                    S.dma("sp", wrb[:, e_, :], wrT[e_:e_ + 1, :].partition_broadcast(128), reads=[r_wrTd], writes=[r_wrb, r_wr, r_wrTs])
            G5 = 256
            wos = []
            if not last:
                wo_c = AR.alloc([8, D], BF16); r_woc = Res("woc", multi=True)
                for k in range(8):
                    S.op("dve", f_tt(wo_c[:, k, :], wo[:, k, :], m5[1][0], ALU.mult), reads=[r_wm, m5[1][3]], writes=[r_woc])
            r_wos = Res("wos", multi=True)
            for k in range(8):
                S.op("dve", f_tt(wo[:, k, :], wo[:, k, :], m5[0][0], ALU.mult), reads=[r_wm, m5[0][3]] + ([r_woc] if not last else []), writes=[r_wos, r_wm])
            wos.append((wo, r_wos))
            if not last:
                wos.append((wo_c, r_woc))
            XTs = Ring([(AR.alloc([4, G5], BF16), Res("XTs%d" % i)) for i in range(2)])
            ATs = Ring([(AR.alloc([4, G5], BF16), Res("ATs%d" % i)) for i in range(2)])
            CTs = Ring([(AR.alloc([2, G5], BF16), Res("CTs%d" % i)) for i in range(2)])
            GTs = Ring([(AR.alloc([24, G5], BF16), Res("GTs%d" % i, multi=True)) for i in range(2)])
            sTs = Ring([(AR.alloc([8, G5], BF16), Res("sTs%d" % i, multi=True)) for i in range(2)])
            t1s = Ring([(AR.alloc([3, G5], F32), Res("t1s%d" % i, multi=True)) for i in range(2)])
            xrs = Ring([(AR.alloc([D], F32), Res("xr%d" % i)) for i in range(2)])
            xms = Ring([(AR.alloc([D], F32), Res("xm%d" % i, multi=True)) for i in range(2)])
            tmp5 = AR.alloc([D], F32); r_tmp5 = Res("tmp5")
            ss5 = AR.alloc([1], F32); r_ss5 = Res("ss5")
            hfs = Ring([(AR.alloc([D], F32), Res("hf%d" % i)) for i in range(2)])
            hb5 = Ring([AR.alloc([D], BF16) for i in range(2)])
            h2s = Ring([(AR.alloc([8, G5], BF16), Res("h2s%d" % i, multi=True)) for i in range(2)])
            lg = Ring([(AR.alloc([24], F32), Res("lg%d" % i)) for i in range(2)])
            if not last:
                groups5a = [(g0, G5, 0) for g0 in range(0, T, G5)] + [(T, TC, 1)]
                groups5 = [(g0, 512, 0) for g0 in range(0, T, 512)] + [(T, TC, 1)]
            else:
                groups5a = [(PADR + G5 * j, G5, 0) for j in range(HALF // G5)]
                groups5 = [(512 * j, 512, 0) for j in range(1, 9)]
            def p5_A(g0, gs, si):
                XT_, r_XT = XTs.next(); AT_, r_AT = ATs.next(); CT_, r_CT = CTs.next(); GT_, r_GT = GTs.next()
                if not last:
                    S.dma("sp", XT_[:, :, 0:gs], XTd[:, g0:g0 + gs].rearrange("(j p) t -> p j t", p=128), reads=[R["XT"]], writes=[r_XT])
                else:
                    S.dma("sp", XT_[:, :, 0:gs], XTloc[:, g0 - PADR:g0 - PADR + gs].rearrange("(j p) t -> p j t", p=128), reads=[r_XTloc], writes=[r_XT])
                S.dma("sp", AT_[:, :, 0:gs], attnTd[:, g0:g0 + gs].rearrange("(j p) t -> p j t", p=128), reads=[R["attnT"]], writes=[r_AT])
                S.dma("sp", CT_[:, :, 0:gs], convTd[:, g0:g0 + gs].rearrange("(j p) t -> p j t", p=128), reads=[R["convT"]], writes=[r_CT])
                for i3 in range(3):
                    S.dma("sp", GT_[:, i3 * 8:(i3 + 1) * 8, 0:gs], gTd[i3 * 1024:(i3 + 1) * 1024, g0:g0 + gs].rearrange("(j p) t -> p j t", p=128), reads=[R["gT"]], writes=[r_GT])
                sT, r_sT = sTs.next()
                for cc in range(8):
                    c0 = cc * 128
                    t1, r_t1 = t1s.next()
                    specs = [(wcs, r_wcs, XT_, r_XT, 4, 0), (wna, r_wm, AT_, r_AT, 4, 8), (wco, r_wm, CT_, r_CT, 2, 16)]
                    for bi_, (wt, r_wt, src, r_src, nk_, goff) in enumerate(specs):
                        pb, r_pb = PF.next()
                        S.op("pe", [f_mm(pb[:, 0:gs], wt[:, k, c0:c0 + 128], src[:, k, 0:gs], k == 0, k == nk_ - 1) for k in range(nk_)],
                             reads=[r_wt, r_src], writes=[r_pb])
                        S.op("dve", f_tt(t1[:, bi_, 0:gs], pb[:, 0:gs], GT_[:, goff + cc, 0:gs], ALU.mult), reads=[r_pb, r_GT], writes=[r_t1])
                    S.op("pool", f_tt(t1[:, 0, 0:gs], t1[:, 0, 0:gs], t1[:, 1, 0:gs], ALU.add), reads=[r_t1], writes=[r_t1])
                    S.op("pool", f_tt(sT[:, cc, 0:gs], t1[:, 0, 0:gs], t1[:, 2, 0:gs], ALU.add), reads=[r_t1], writes=[r_sT])
                return (sT, r_sT)

            def p5_B1(g0, gs, si, tt, sTt):
                sT, r_sT = sTt
                gt, gm, sh, r_m = m5[si]
                wo_s, r_wo_s = wos[si]
                tok0 = g0 + tt * 128
                xr, r_xr = xrs.next(); xm, r_xm = xms.next()
                if not last:
                    S.dma("sp", xr, xb[xbrow(tok0):xbrow(tok0) + 128, :], reads=[r_xb], writes=[r_xr])
                else:
                    S.dma("sp", xr, xloc[tok0:tok0 + 128, :], reads=[r_xloc], writes=[r_xr])
                for nh in range(2):
                    pb, r_pb = PF.next()
                    S.op("pe", [f_mm(pb[:, :], sT[:, k, tt * 128:(tt + 1) * 128], wo_s[:, k, nh * 512:(nh + 1) * 512], k == 0, k == 7) for k in range(8)],
                         reads=[r_sT, r_wo_s], writes=[r_pb])
                    S.op("dve", f_tt(xm[:, nh * 512:(nh + 1) * 512], pb[:, :], xr[:, nh * 512:(nh + 1) * 512], ALU.add), reads=[r_pb, r_xr], writes=[r_xm])
                S.dma("sp", xa[tok0:tok0 + 128, :], xm, reads=[r_xm], writes=[r_xa])
                hf, r_hf = hfs.next(); hb = hb5.next()
                norm_tile(xm, r_xm, gm, sh, r_m, hb, r_hf, tmp5, r_tmp5, ss5, r_ss5, out_f32=hf)
                return (tok0, hf, r_hf, hb)

            def p5_B2(tt, st_, h2t):
                tok0, hf, r_hf, hb = st_
                h2, r_h2 = h2t
                if not last:
                    transpose_to(hb, r_hf, h2[:, :, tt * 128:(tt + 1) * 128], r_h2, 8)
                else:
                    S.dma("sp", h2tok[tok0 - PADR:tok0 - PADR + 128, :], hb, reads=[r_hf], writes=[r_h2tok])
                    lgt, r_lg = lg.next()
                    S.op("pool", f_ms(lgt[:, 0:8], 0.0), writes=[r_lg])
                    for e_ in range(8):
                        S.op("dve", f_stt(tmp5, hf, 1.0, wrb[:, e_, :], ALU.mult, ALU.mult, accum_out=lgt[:, e_:e_ + 1]),
                             reads=[r_hf, r_wrb, r_lg], writes=[r_tmp5, r_lg])
                    S.op("dve", (lambda o, i: (lambda e: e.max(out=o, in_=i)))(lgt[:, 8:16], lgt[:, 0:8]), reads=[r_lg], writes=[r_lg])
                    S.op("dve", (lambda o, a, s1: (lambda e: e.tensor_scalar(out=o, in0=a, scalar1=s1, scalar2=-80.0, op0=ALU.subtract, op1=ALU.max)))(lgt[:, 16:24], lgt[:, 0:8], lgt[:, 8:9]),
                         reads=[r_lg], writes=[r_lg])
                    S.op("act", f_act(lgt[:, 16:24], lgt[:, 16:24], AF.Exp), reads=[r_lg], writes=[r_lg])
                    S.op("dve", f_stt(lgt[:, 16:24], lgt[:, 0:8], lgt[:, 9:10], lgt[:, 16:24], ALU.is_ge, ALU.mult), reads=[r_lg], writes=[r_lg])
                    S.op("dve", (lambda o, i: (lambda e: e.reduce_sum(out=o, in_=i, axis=mybir.AxisListType.X)))(lgt[:, 8:9], lgt[:, 16:24]), reads=[r_lg], writes=[r_lg])
                    S.op("dve", f_rec(lgt[:, 8:9], lgt[:, 8:9]), reads=[r_lg], writes=[r_lg])
                    S.op("dve", f_ts(Gall[:, (tok0 - PADR) // 128, :], lgt[:, 16:24], lgt[:, 8:9], ALU.mult), reads=[r_lg], writes=[r_Gall])

            sT_next = p5_A(*groups5a[0])
            for gi, (g0, gs, si) in enumerate(groups5a):
                sT_cur = sT_next
                if gi + 1 < len(groups5a):
                    sT_next = p5_A(*groups5a[gi + 1])
                h2t = h2s.next()
                ntl = gs // 128
                sts = {}
                for tt in range(ntl):
                    sts[tt] = p5_B1(g0, gs, si, tt, sT_cur)
                    if tt >= 1:
                        p5_B2(tt - 1, sts.pop(tt - 1), h2t)
                p5_B2(ntl - 1, sts.pop(ntl - 1), h2t)
                if not last:
                    S.dma("sp", h2Td[:, g0:g0 + gs].rearrange("(k p) t -> p k t", p=128), h2t[0][:, :, 0:gs], reads=[h2t[1]], writes=[R["h2T"]])

            if stop == (l, "P5b"):
                raise _StopBuild()
            S.barrier(); AR.reset()
            if last:
                AXX = mybir.AxisListType.X
                def f_tsc(o, a, s1, op0):
                    return lambda e: e.tensor_scalar(out=o, in0=a, scalar1=s1, scalar2=None, op0=op0)

                def f_rs(o, i):
                    return lambda e: e.reduce_sum(out=o, in_=i, axis=AXX)

                def f_cpd(o, i):
                    return lambda e: e.tensor_copy(out=o, in_=i)

                def f_iota(o, pat, cm):
                    return lambda e: e.iota(o, pattern=pat, base=0, channel_multiplier=cm)
                r_md = Res("md")
                Mk = AR.alloc([32, 8], F32); PA = AR.alloc([32, 8], F32); PBt = AR.alloc([32, 8], F32)
                rank = AR.alloc([32, 8], F32); cum = AR.alloc([32, 8], F32); F1 = AR.alloc([32, 8], F32); F2 = AR.alloc([32, 8], F32)
                tmp3 = AR.alloc([32, 8], F32)
                totb = AR.alloc([8], BF16); ustr = AR.alloc([128], BF16); uones = AR.alloc([128], BF16); ustf = AR.alloc([128], F32)
                offn = AR.alloc([2, 8], F32)
                thr_i = AR.alloc([9], I32); thr = AR.alloc([9], F32); cmp9 = AR.alloc([8, 9], F32)
                cnt = AR.alloc([8], F32); Gs = AR.alloc([8], F32); ends = AR.alloc([8], F32); sbs = AR.alloc([8], F32)
                gio_i = AR.alloc([NGRP], I32); gio = AR.alloc([NGRP], F32); cmp2 = AR.alloc([NGRP, 8], F32); ge = AR.alloc([NGRP], F32)
                kp_i = AR.alloc([8], I32); kp = AR.alloc([8], F32); jp_i = AR.alloc([28], I32); jp = AR.alloc([28], F32)
                geg = AR.alloc([NGRP], F32); ged = AR.alloc([NGRP], F32)
                idxGUf = AR.alloc([NGRP, 8], F32); idxGU = AR.alloc([NGRP, 8], I32)
                idxDf = AR.alloc([NGRP, 28], F32); idxD = AR.alloc([NGRP, 28], I32)
                d12f = AR.alloc([2, 32], F32)

                def MD(eng, fn, extra_r=()):
                    S.op(eng, fn, reads=[r_md] + list(extra_r), writes=[r_md])
                MD("dve", f_tsc(Mk, Gall[:], 0.0, ALU.is_gt), [r_Gall])
                MD("dve", f_cpd(PA, Mk))
                src_, dst_ = PA, PBt
                for sft in (1, 2, 4, 8, 16):
                    MD("dve", f_cpd(dst_[:, 0:sft, :], src_[:, 0:sft, :]))
                    MD("dve", f_tt(dst_[:, sft:32, :], src_[:, sft:32, :], src_[:, 0:32 - sft, :], ALU.add))
                    src_, dst_ = dst_, src_
                incl = src_
                MD("dve", f_cpd(totb, incl[:, 31, :]))
                MD("pool", f_ms(ustf, 1.0))
                MD("pool", lambda e: e.affine_select(out=ustf, in_=ustf, pattern=[[1, 128]], compare_op=ALU.is_gt, fill=0.0, base=0, channel_multiplier=-1))
                MD("dve", f_cpd(ustr, ustf))
                MD("pool", f_ms(uones, 1.0))
                pbm, r_pbm = PF.next()
                S.op("pe", [f_mm(pbm[:, 0:8], ustr, totb, True, True), f_mm(pbm[:, 8:16], uones, totb, True, True)], reads=[r_md], writes=[r_pbm])
                S.op("dve", f_cpd(offn, pbm[:, 0:16].rearrange("p (a e) -> p a e", e=8)), reads=[r_pbm, r_md], writes=[r_md])
                MD("dve", f_tt(rank, incl, Mk, ALU.subtract))
                MD("dve", f_tt(rank, rank, offn[:, 0:1, :].to_broadcast([128, 32, 8]), ALU.add))
                MD("pool", f_iota(thr_i, [[512, 9]], 0))
                MD("dve", f_cpd(thr, thr_i))
                MD("dve", f_tt(cmp9, offn[:, 1, :].unsqueeze(2).to_broadcast([128, 8, 9]), thr.unsqueeze(1).to_broadcast([128, 8, 9]), ALU.is_gt))
                MD("dve", f_rs(cnt, cmp9))
                MD("pool", f_ms(Gs[:, 0:1], 0.0))
                for e_ in range(1, 8):
                    MD("dve", f_tt(Gs[:, e_:e_ + 1], Gs[:, e_ - 1:e_], cnt[:, e_ - 1:e_], ALU.add))
                MD("dve", f_tt(ends, Gs, cnt, ALU.add))
                MD("dve", f_tsc(sbs, Gs, 512.0, ALU.mult))
                MD("dve", f_tt(rank, rank, sbs.unsqueeze(1).to_broadcast([128, 32, 8]), ALU.add))
                MD("pool", f_ms(cum[:, :, 0:1], 0.0))
                for e_ in range(1, 8):
                    MD("dve", f_tt(cum[:, :, e_:e_ + 1], cum[:, :, e_ - 1:e_], Mk[:, :, e_ - 1:e_], ALU.add))
                MD("dve", f_stt(F1, cum, 0.0, Mk, ALU.is_equal, ALU.mult))
                MD("dve", f_stt(F2, cum, 1.0, Mk, ALU.is_equal, ALU.mult))
                for ki, Fk in enumerate((F1, F2)):
                    MD("dve", f_tt(tmp3, Fk, rank, ALU.mult))
                    MD("dve", f_rs(d12f[:, ki, :], tmp3))
                    MD("dve", f_tt(tmp3, Fk, Gall[:], ALU.mult), [r_Gall])
                    S.op("dve", f_rs(g12[:, ki, :], tmp3), reads=[r_md], writes=[r_md, r_route])
                S.op("dve", f_cpd(d12i[:], d12f), reads=[r_md], writes=[r_md, r_route])
                MD("pool", f_iota(gio_i, [[1, NGRP]], 0))
                MD("dve", f_cpd(gio, gio_i))
                MD("dve", f_tt(cmp2, ends.unsqueeze(1).to_broadcast([128, NGRP, 8]), gio.unsqueeze(2).to_broadcast([128, NGRP, 8]), ALU.is_le))
                MD("dve", f_rs(ge, cmp2))
                MD("dve", lambda e: e.tensor_scalar_min(out=ge, in0=ge, scalar1=7.0))
                MD("pool", f_iota(kp_i, [[128, 8]], 1))
                MD("dve", f_cpd(kp, kp_i))
                MD("pool", f_iota(jp_i, [[128, 28]], 1))
                MD("dve", f_cpd(jp, jp_i))
                MD("dve", f_tsc(geg, ge, float(D), ALU.mult))
                MD("dve", f_tsc(ged, ge, 3584.0, ALU.mult))
                MD("dve", f_tt(idxGUf, geg.unsqueeze(2).to_broadcast([128, NGRP, 8]), kp.unsqueeze(1).to_broadcast([128, NGRP, 8]), ALU.add))
                MD("dve", f_cpd(idxGU, idxGUf))
                MD("dve", f_tt(idxDf, ged.unsqueeze(2).to_broadcast([128, NGRP, 28]), jp.unsqueeze(1).to_broadcast([128, NGRP, 28]), ALU.add))
                MD("dve", f_cpd(idxD, idxDf))

                r_Hs = Res("Hs", multi=True); r_Ys = Res("Ys", multi=True)
                hts = Ring([(AR.alloc([D], BF16), Res("ht%d" % i)) for i in range(3)])

                def f_scatter(dst, idx_ap, src):
                    return lambda e: e.indirect_dma_start(out=dst, out_offset=bass.IndirectOffsetOnAxis(ap=idx_ap, axis=0), in_=src, in_offset=None)

                def f_gather(dst, src, idx_ap):
                    return lambda e: e.indirect_dma_start(out=dst, out_offset=None, in_=src, in_offset=bass.IndirectOffsetOnAxis(ap=idx_ap, axis=0))
                for a in range(32):
                    ht, r_ht = hts.next()
                    S.dma("sp", ht, h2tok[a * 128:(a + 1) * 128, :], reads=[r_h2tok], writes=[r_ht])
                    for ki in range(2):
                        S.dma_fn("pool", f_scatter(Hs[:, :], d12i[:, ki, a:a + 1].bitcast(U32), ht), reads=[r_ht, r_route], writes=[r_Hs])

                hss = Ring([(AR.alloc([D], BF16), Res("hs%d" % i)) for i in range(2)])
                hTg = Ring([(AR.alloc([8, 512], BF16), Res("hTg%d" % i, multi=True)) for i in range(2)])
                wgus = Ring([(AR.alloc([8, 1792], BF16), Res("wgu%d" % i, multi=True)) for i in range(2)])
                wdqs = Ring([(AR.alloc([7, D], BF16), Res("wdq%d" % i, multi=True)) for i in range(2)])
                aTqs = Ring([(AR.alloc([7, 512], BF16), Res("aTq%d" % i, multi=True)) for i in range(2)])
                sgs = Ring([(AR.alloc([512], BF16), Res("sg%d" % i)) for i in range(3)])
                accs = Ring([(AR.alloc([4, D], F32), Res("acc%d" % i, multi=True)) for i in range(1)])
                mwd2 = mwd.rearrange("e f n -> (e f) n")
                for g in range(NGRP):
                    hT, r_hT = hTg.next()
                    for tt in range(4):
                        hs_, r_hs = hss.next()
                        S.dma("sp", hs_, Hs[g * 512 + tt * 128:g * 512 + (tt + 1) * 128, :], reads=[r_Hs], writes=[r_hs])
                        transpose_to(hs_, r_hs, hT[:, :, tt * 128:(tt + 1) * 128], r_hT, 8)
                    acc, r_acc = accs.next()
                    for q in range(4):
                        wgu, r_wgu = wgus.next(); wdq, r_wdq = wdqs.next(); aTq, r_aTq = aTqs.next()
                        for k in range(8):
                            S.dma_fn("pool", f_gather(wgu[:, k, :], mwgu[q][:, :], idxGU[:, g, k:k + 1].bitcast(U32)), reads=[r_mw, r_md], writes=[r_wgu])
                        for j in range(7):
                            S.dma_fn("pool", f_gather(wdq[:, j, :], mwd2, idxD[:, g, q * 7 + j:q * 7 + j + 1].bitcast(U32)), reads=[r_mw, r_md], writes=[r_wdq])
                        for c in range(7):
                            pg, r_pg = PF.next(); pu, r_pu = PF.next()
                            S.op("pe", [f_mm(pg[:, :], wgu[:, k, c * 128:(c + 1) * 128], hT[:, k, :], k == 0, k == 7) for k in range(8)], reads=[r_wgu, r_hT], writes=[r_pg])
                            S.op("pe", [f_mm(pu[:, :], wgu[:, k, 896 + c * 128:896 + (c + 1) * 128], hT[:, k, :], k == 0, k == 7) for k in range(8)], reads=[r_wgu, r_hT], writes=[r_pu])
                            sg, r_sg = sgs.next()
                            S.op("act", f_act(sg, pg[:, :], AF.Silu), reads=[r_pg], writes=[r_sg])
                            S.op("dve", f_tt(aTq[:, c, :], pu[:, :], sg, ALU.mult), reads=[r_pu, r_sg], writes=[r_aTq])
                        for tt in range(4):
                            for nh in range(2):
                                pd, r_pd = PF.next()
                                S.op("pe", [f_mm(pd[:, :], aTq[:, c, tt * 128:(tt + 1) * 128], wdq[:, c, nh * 512:(nh + 1) * 512], c == 0, c == 6) for c in range(7)],
                                     reads=[r_aTq, r_wdq], writes=[r_pd])
                                ao = acc[:, tt, nh * 512:(nh + 1) * 512]
                                if q == 0:
                                    copy_op(evac_eng(), ao, pd[:, :], [r_pd], [r_acc])
                                else:
                                    S.op("dve", f_tt(ao, pd[:, :], ao, ALU.add), reads=[r_pd, r_acc], writes=[r_acc])
                    for tt in range(4):
                        S.dma("sp", Ys[g * 512 + tt * 128:g * 512 + (tt + 1) * 128, :], acc[:, tt, :], reads=[r_acc], writes=[r_Ys])

                S.barrier(); AR.reset()
                g2t = AR.alloc([D], F32); r_g2t = Res("g2t")
                S.dma("sp", g2t, modd[0:1, 5 * D:6 * D].partition_broadcast(128), reads=[R["modd"]], writes=[r_g2t])
                fg = AR.alloc([D], F32); r_fg = Res("fg")
                S.dma("sp", fg, final_g[0:1, :].partition_broadcast(128), writes=[r_fg])
                y1s = Ring([(AR.alloc([D], F32), Res("y1%d" % i)) for i in range(2)])
                y2s = Ring([(AR.alloc([D], F32), Res("y2%d" % i)) for i in range(2)])
                xm5 = Ring([(AR.alloc([D], F32), Res("xm5%d" % i)) for i in range(2)])
                xo5 = Ring([(AR.alloc([D], F32), Res("xo5%d" % i)) for i in range(2)])
                tmp6 = AR.alloc([D], F32); r_tmp6 = Res("tmp6")
                ss6 = AR.alloc([1], F32); r_ss6 = Res("ss6")
                for a in range(32):
                    tok0 = PADR + a * 128
                    y1, r_y1 = y1s.next(); y2, r_y2 = y2s.next()
                    S.dma_fn("pool", f_gather(y1, Ys[:, :], d12i[:, 0, a:a + 1].bitcast(U32)), reads=[r_Ys, r_route], writes=[r_y1])
                    S.dma_fn("pool", f_gather(y2, Ys[:, :], d12i[:, 1, a:a + 1].bitcast(U32)), reads=[r_Ys, r_route], writes=[r_y2])
                    xm, r_xm = xm5.next(); xo, r_xo = xo5.next()
                    S.dma("sp", xm, xa[tok0:tok0 + 128, :], reads=[r_xa], writes=[r_xm])
                    S.op("dve", f_ts(xo, y1, g12[:, 0, a:a + 1], ALU.mult), reads=[r_y1, r_route], writes=[r_xo])
                    S.op("dve", f_stt(xo, y2, g12[:, 1, a:a + 1], xo, ALU.mult, ALU.add), reads=[r_y2, r_route, r_xo], writes=[r_xo])
                    S.op("pool", f_tt(xo, xo, g2t, ALU.mult), reads=[r_xo, r_g2t], writes=[r_xo])
                    S.op("pool", f_tt(xo, xo, xm, ALU.add), reads=[r_xo, r_xm], writes=[r_xo])
                    S.op("act", f_act(tmp6, xo, AF.Square, accum_out=ss6), reads=[r_xo], writes=[r_tmp6, r_ss6])
                    S.op("act", f_act(ss6, ss6, AF.Sqrt, bias=epsb[:], scale=1.0 / D), reads=[r_ss6, r_const], writes=[r_ss6])
                    S.op("dve", f_rec(ss6, ss6), reads=[r_ss6], writes=[r_ss6])
                    S.op("dve", f_stt(xo, xo, ss6, fg, ALU.mult, ALU.mult), reads=[r_xo, r_ss6, r_fg], writes=[r_xo])
                    S.dma("sp", y_out[a * 128:(a + 1) * 128, :], xo, reads=[r_xo])
                continue
            nexp = 8 if last else 1
            nch = 28 if last else 22
            BL = 2
            nblk = nch // BL
            gt2s = []
            for (t0, tl, row) in seqs5:
                g_ = AR.alloc([D], F32); r_ = Res("gt2_%d" % row)
                S.dma("sp", g_, modd[row:row + 1, 5 * D:6 * D].partition_broadcast(128), reads=[R["modd"]], writes=[r_])
                gt2s.append((g_, r_))
            if last:
                fg = AR.alloc([D], F32); r_fg = Res("fg")
                S.dma("sp", fg, final_g[0:1, :].partition_broadcast(128), writes=[r_fg])
            h2g = Ring([(AR.alloc([8, 512], BF16), Res("h2g%d" % i)) for i in range(2)])
            wgs = Ring([(AR.alloc([8, BL * 128], BF16), Res("wgs%d" % i)) for i in range(3)])
            wus = Ring([(AR.alloc([8, BL * 128], BF16), Res("wus%d" % i)) for i in range(3)])
            wds = [(AR.alloc([BL, D], BF16), Res("wds%d" % i)) for i in range(nblk)]
            aTb = AR.alloc([nch, 512], BF16); r_aT5 = Res("aT5", multi=True)
            sgs = Ring([(AR.alloc([512], BF16), Res("sg%d" % i)) for i in range(3)])
            accs = Ring([(AR.alloc([4, D], F32), Res("acc%d" % i, multi=True)) for i in range(1)])
            gts = Ring([(AR.alloc([4, 8], F32), Res("gts%d" % i)) for i in range(2)])
            xm5 = Ring([(AR.alloc([D], F32), Res("xm5%d" % i)) for i in range(2)])
            xo5 = Ring([(AR.alloc([D], F32), Res("xo5%d" % i)) for i in range(2)])
            tmp6 = AR.alloc([D], F32); r_tmp6 = Res("tmp6")
            ss6 = AR.alloc([1], F32); r_ss6 = Res("ss6")
            r_wsrc = r_mw if last else r_fw
            gtl = r_gtl = None
            for (g0, gs, si) in groups5:
                if True:
                    g2t, r_g2t = gt2s[si]
                    hg, r_hg = h2g.next()
                    S.dma("sp", hg[:, :, 0:gs], h2Td[:, g0:g0 + gs].rearrange("(k p) t -> p k t", p=128), reads=[R["h2T"]], writes=[r_hg])
                    acc, r_acc = accs.next()
                    if last:
                        gtl, r_gtl = gts.next()
                        S.dma("sp", gtl[:, 0:gs // 128, :], gatesd[g0:g0 + gs, :].rearrange("(a p) e -> p a e", p=128), reads=[R["gates"]], writes=[r_gtl])
                    for ex in range(nexp):
                        if last:
                            Wg, Wu, Wd = mwg[ex], mwu[ex], mwd[ex]
                        else:
                            Wg, Wu, Wd = fwg, fwu, fwd
                        for b in range(nblk):
                            f0 = b * BL * 128
                            wg_, r_wg = wgs.next(); wu_, r_wu = wus.next(); wd_, r_wd = wds[b]
                            S.dma("sp", wg_, Wg[b].rearrange("p (k f) -> p k f", k=8), reads=[r_wsrc], writes=[r_wg])
                            S.dma("sp", wu_, Wu[b].rearrange("p (k f) -> p k f", k=8), reads=[r_wsrc], writes=[r_wu])
                            S.dma("pool", wd_, Wd[f0:f0 + BL * 128, :].rearrange("(j p) n -> p j n", p=128), reads=[r_wsrc], writes=[r_wd])
                            for j in range(BL):
                                ch = b * BL + j
                                pg, r_pg = PF.next(); pu, r_pu = PF.next()
                                S.op("pe", [f_mm(pg[:, 0:gs], wg_[:, k, j * 128:(j + 1) * 128], hg[:, k, 0:gs], k == 0, k == 7) for k in range(8)],
                                     reads=[r_wg, r_hg], writes=[r_pg])
                                S.op("pe", [f_mm(pu[:, 0:gs], wu_[:, k, j * 128:(j + 1) * 128], hg[:, k, 0:gs], k == 0, k == 7) for k in range(8)],
                                     reads=[r_wu, r_hg], writes=[r_pu])
                                sg, r_sg = sgs.next()
                                S.op("act", f_act(sg[:, 0:gs], pg[:, 0:gs], AF.Silu), reads=[r_pg], writes=[r_sg])
                                S.op("dve", f_tt(aTb[:, ch, 0:gs], pu[:, 0:gs], sg[:, 0:gs], ALU.mult), reads=[r_pu, r_sg], writes=[r_aT5])
                        for tt in range(gs // 128):
                            for nh in range(2):
                                pd, r_pd = PF.next()
                                S.op("pe", [f_mm(pd[:, :], aTb[:, ch, tt * 128:(tt + 1) * 128], wds[ch // BL][0][:, ch % BL, nh * 512:(nh + 1) * 512], ch == 0, ch == nch - 1) for ch in range(nch)],
                                     reads=[r_aT5] + [w_[1] for w_ in wds], writes=[r_pd])
                                ao = acc[:, tt, nh * 512:(nh + 1) * 512]
                                if not last:
                                    copy_op(evac_eng(), ao, pd[:, :], [r_pd], [r_acc])
                                elif ex == 0:
                                    S.op("dve", f_ts(ao, pd[:, :], gtl[:, tt, ex:ex + 1], ALU.mult), reads=[r_pd, r_gtl], writes=[r_acc])
                                else:
                                    S.op("dve", f_stt(ao, pd[:, :], gtl[:, tt, ex:ex + 1], ao, ALU.mult, ALU.add), reads=[r_pd, r_gtl, r_acc], writes=[r_acc])
                    for tt in range(gs // 128):
                        tok0 = g0 + tt * 128
                        xm, r_xm = xm5.next(); xo, r_xo = xo5.next()
                        S.dma("sp", xm, xa[tok0:tok0 + 128, :], reads=[r_xa], writes=[r_xm])
                        S.op("dve", f_tt(xo, acc[:, tt, :], g2t, ALU.mult), reads=[r_acc, r_g2t], writes=[r_xo])
                        S.op("pool", f_tt(xo, xo, xm, ALU.add), reads=[r_xo, r_xm], writes=[r_xo])
                        if not last:
                            S.dma("sp", xb[xbrow(tok0):xbrow(tok0) + 128, :], xo, reads=[r_xo], writes=[r_xb])
                            if dbg:
                                S.dma("sp", dbgs["xb1"][xbrow(tok0):xbrow(tok0) + 128, :], xo, reads=[r_xo])
                        else:
                            S.op("act", f_act(tmp6, xo, AF.Square, accum_out=ss6), reads=[r_xo], writes=[r_tmp6, r_ss6])
                            S.op("act", f_act(ss6, ss6, AF.Sqrt, bias=epsb[:], scale=1.0 / D), reads=[r_ss6, r_const], writes=[r_ss6])
                            S.op("dve", f_rec(ss6, ss6), reads=[r_ss6], writes=[r_ss6])
                            S.op("dve", f_stt(xo, xo, ss6, fg, ALU.mult, ALU.mult), reads=[r_xo, r_ss6, r_fg], writes=[r_xo])
                            S.dma("sp", y_out[tok0 - PADR:tok0 - PADR + 128, :], xo, reads=[r_xo])
            if dbg and not last:
                for i in range(66):
                    S.dma("sp", dbgs["xa0"][i * 128:(i + 1) * 128, :], xa[i * 128:(i + 1) * 128, :], reads=[r_xa])
        except _StopBuild:
            pass
        S.barrier()
        for dn, dst_ in dump_out.items():
            src_ = scr_by_name[dn]
            if len(src_.shape) == 3:
                for i_ in range(src_.shape[0]):
                    S.dma("sp", dst_[i_], src_[i_])
            else:
                n0 = src_.shape[0]
                step = max(1, n0 // 8)
                for i_ in range(0, n0, step):
                    S.dma("sp", dst_[i_:min(n0, i_ + step)], src_[i_:min(n0, i_ + step)])
        S.barrier()
        S.emit()
    return nc


def _bf16(a):
    return np.asarray(a, dtype=np.float32).astype(ml_dtypes.bfloat16)


def _fill_bias(out_v, rpb_l, r0, krow0, nch):
    cs = np.clip(np.arange(64) - 8, 0, 48)
    for qi in range(2):
        r = r0 + qi
        start = min(max(r - 4, 0), 120)
        for j in range(nch):
            for kp in range(2):
                krow = krow0 + 2 * j + kp
                if not (start <= krow < start + 8):
                    continue
                dr = krow - r + 7
                for c in range(64):
                    kcs = np.arange(cs[c], cs[c] + 16)
                    dc = kcs - c + 15
                    out_v[kp * 64 + kcs, :, j, qi * 64 + c] = rpb_l[:, dr, dc].T


def _bias_tables_l0(rpb_l):
    out = np.full((5, 128, 8, 5, 128), NEG, np.float32)
    for v, m in {0: 4, 1: 0, 2: 1, 3: 62, 4: 63}.items():
        r0 = 2 * m
        _fill_bias(out[v], rpb_l, r0, min(max(r0 - 4, 0), 118), 5)
    return out.reshape(5, 128, 8 * 5 * 128)


def _bias_tables_l1(rpb_l, h):
    out = np.full((5, 128, 8, 6, 128), NEG, np.float32)
    for v, ml in {0: 8, 1: 0, 2: 1, 3: 30, 4: 31}.items():
        r0 = 2 * (ml + 32 * h)
        _fill_bias(out[v], rpb_l, r0, r0 - (6 if ml == 31 else 4), 6)
    return out.reshape(5, 128, 8 * 6 * 128)


def _tables():
    Ls = 8192
    l1 = np.arange(128)[:, None, None]
    l0 = np.arange(64)[None, :, None]
    k1 = np.arange(128)[None, None, :]
    ang = 2 * np.pi * ((k1 * (64 * l1 + l0)) % Ls) / Ls
    sc = 1.0 / np.sqrt(Ls)
    dftA = np.stack([np.cos(ang) * sc, -np.sin(ang) * sc], axis=2)
    a64 = 2 * np.pi * (np.arange(64)[:, None] * np.arange(64)[None, :] % 64) / 64
    dft64 = np.stack([np.cos(a64), np.sin(a64), -np.sin(a64)], axis=1)
    a256 = 2 * np.pi * (np.arange(256)[:, None] * np.arange(256)[None, :] % 256) / 256
    dftc = np.stack([np.cos(a256) / 16.0, -np.sin(a256) / 16.0], axis=1)
    cb = np.zeros((256, 2, 256), np.float64)
    for g in range(4):
        cb[g * 64:(g + 1) * 64, 0, g * 64:(g + 1) * 64] = np.cos(a64) / 8.0
        cb[g * 64:(g + 1) * 64, 1, g * 64:(g + 1) * 64] = np.sin(a64) / 8.0
    return (_bf16(dftA.reshape(128, -1)), _bf16(dft64.reshape(64, -1)), _bf16(dftc.reshape(256, -1)), _bf16(cb.reshape(256, -1)))


_NC_CACHE = {}


def kernel(x, c, ctx, c_ctx, norm1_g, norm2_g, w_ada, b_ada, w_in, conv_w, na_rpb, w_fourier, w_na, w_conv_out,
           w_out, ffn_w_gate, ffn_w_up, ffn_w_down, moe_router, moe_w_gate, moe_w_up, moe_w_down, final_g, _dbg=False, _stop=None, _dumps=(), _nb=None):
    f = lambda a: np.ascontiguousarray(np.asarray(a, dtype=np.float32))
    x = f(x); c = f(c); ctx = f(ctx); c_ctx = f(c_ctx)
    B = x.shape[0]
    dftA, dft64, dftc, c64bd = _tables()
    rpb = f(na_rpb)
    biasT = _bf16(_bias_tables_l0(rpb[0]))
    biasT1 = [_bf16(_bias_tables_l1(rpb[1], h)) for h in range(2)]
    emask = [np.array([[0.0, 1.0]], np.float32), np.array([[1.0, 0.0]], np.float32)]
    shared = {
        "norm1_g": f(norm1_g), "norm2_g": f(norm2_g), "w_ada": f(w_ada), "b_ada": f(b_ada), "w_in": f(w_in),
        "conv_w": f(conv_w), "w_fourier": f(w_fourier), "w_na": f(w_na), "w_conv_out": f(w_conv_out), "w_out": f(w_out),
        "ffn_w_gate": f(ffn_w_gate), "ffn_w_up": f(ffn_w_up), "ffn_w_down": f(ffn_w_down), "moe_router": f(moe_router),
        "moe_w_gate": f(moe_w_gate), "moe_w_up": f(moe_w_up), "moe_w_down": f(moe_w_down), "final_g": f(final_g).reshape(1, D),
        "biasT": biasT, "dftA": dftA, "dft64": dft64, "dftc": dftc, "c64bd": c64bd,
    }
    key = (bool(_dbg), _stop, tuple(_dumps))
    if key not in _NC_CACHE:
        _NC_CACHE[key] = build_program(dbg=bool(_dbg), stop=_stop, dumps=tuple(_dumps))
    nc = _NC_CACHE[key]
    if _nb is not None:
        B = _nb
    in_maps = []
    for b in range(B):
        for h in range(2):
            m = dict(shared)
            m["x"] = x[b]
            m["ctx"] = ctx[b]
            m["cvec"] = np.ascontiguousarray(np.stack([c[b], c_ctx], axis=0))
            m["biasT1"] = biasT1[h]
            m["edgemask"] = emask[h]
            in_maps.append(m)
    res = run_bass_kernel_spmd(nc, in_maps, core_ids=list(range(2 * B)))
    out = np.stack([np.concatenate([np.asarray(res.results[2 * b + h]["y"], dtype=np.float32) for h in range(2)], axis=0) for b in range(B)], axis=0)
    if _dbg or _dumps:
        return out, res
    return out
```

```python
import numpy as np
import ml_dtypes
from contextlib import ExitStack
import concourse.bass as bass
import concourse.mybir as mybir
from concourse.bass_utils import run_bass_kernel_spmd

F32 = mybir.dt.float32
BF16 = mybir.dt.bfloat16
ALU = mybir.AluOpType
AF = mybir.ActivationFunctionType

ENGS = ("pe", "act", "dve", "pool", "sp")
EPOCH = 30000
NDMA = 14

D = 1024
T = 8192
TC = 256
TT = T + TC
PW = 5632
NEG = -30000.0
PADR = 512
CT0 = PADR + T + PADR
XB_ROWS = CT0 + TC
HALF = T // 2
LT = HALF + 2 * PADR


def xbrow(tok):
    return PADR + tok if tok < T else CT0 + (tok - T)


class Res:
    __slots__ = ("name", "w", "r", "multi")

    def __init__(self, name="", multi=False):
        self.name = name
        self.w = {}
        self.r = {}
        self.multi = multi


class Sched:
    def __init__(self, nc, es):
        self.nc = nc
        self.es = es
        self.q = {e: [] for e in ENGS}
        self.cnt = {e: 0 for e in ENGS}
        self.seen = {e: {} for e in ENGS}
        self.sems = {}
        self.dma_i = {e: 0 for e in ENGS}
        self.last = {}

    def sem(self, key):
        s = self.sems.get(key)
        if s is None:
            s = self.es.enter_context(self.nc.semaphore("s_%s" % "_".join(str(k) for k in key)))
            self.sems[key] = s
        return s

    def _deps(self, eng, reads, writes):
        deps = {}

        def add(k, v):
            if deps.get(k, 0) < v:
                deps[k] = v
        for r in reads:
            for k, v in r.w.items():
                add(k, v)
        for w in writes:
            for k, v in w.r.items():
                add(k, v)
            if (not w.multi) or w.r:
                for k, v in w.w.items():
                    add(k, v)
        out = []
        seen = self.seen[eng]
        for k, v in deps.items():
            if seen.get(k, 0) < v:
                seen[k] = v
                out.append((self.sem(k), v))
        return out

    def _mark(self, tok, reads, writes):
        k, v = tok
        self.last[k] = max(self.last.get(k, 0), v)
        for r in reads:
            if r.r.get(k, 0) < v:
                r.r[k] = v
        for w in writes:
            if w.multi and not w.r:
                if w.w.get(k, 0) < v:
                    w.w[k] = v
            else:
                w.w = {k: v}
                w.r = {}

    def op(self, eng, fns, reads=(), writes=()):
        if not isinstance(fns, (list, tuple)):
            fns = [fns]
        waits = self._deps(eng, reads, writes)
        self.cnt[eng] += 1
        if self.cnt[eng] % EPOCH == 0:
            self.cnt[eng] += 1
        n = self.cnt[eng]
        key = (eng, n // EPOCH)
        val = n % EPOCH
        self.q[eng].append((waits, fns, self.sem(key), 1))
        tok = (key, val)
        self._mark(tok, reads, writes)
        return tok

    def dma(self, eng, out, in_, reads=(), writes=(), **kw):
        i = self.dma_i[eng]
        self.dma_i[eng] += 1
        slot = i % NDMA
        key = ("dma", eng, slot)
        val = 16 * (i // NDMA + 1)
        sem = self.sem(key)
        waits = self._deps(eng, reads, writes)
        if i >= NDMA:
            pv = 16 * (i // NDMA)
            if self.seen[eng].get(key, 0) < pv:
                self.seen[eng][key] = pv
                waits.append((sem, pv))
        fn = lambda e, out=out, in_=in_, kw=kw: e.dma_start(out=out, in_=in_, **kw)
        self.q[eng].append((waits, [fn], sem, 16))
        tok = (key, val)
        self._mark(tok, reads, writes)
        return tok

    def dma_fn(self, eng, fn, reads=(), writes=()):
        i = self.dma_i[eng]
        self.dma_i[eng] += 1
        slot = i % NDMA
        key = ("dma", eng, slot)
        val = 16 * (i // NDMA + 1)
        sem = self.sem(key)
        waits = self._deps(eng, reads, writes)
        if i >= NDMA:
            pv = 16 * (i // NDMA)
            if self.seen[eng].get(key, 0) < pv:
                self.seen[eng][key] = pv
                waits.append((sem, pv))
        self.q[eng].append((waits, [fn], sem, 16))
        tok = (key, val)
        self._mark(tok, reads, writes)
        return tok

    def barrier(self):
        for eng in ENGS:
            waits = []
            for k, v in self.last.items():
                if self.seen[eng].get(k, 0) < v:
                    self.seen[eng][k] = v
                    waits.append((self.sem(k), v))
            if waits:
                self.q[eng].append((waits, [], None, 0))

    def emit(self):
        with self.nc.Block() as block:
            def runner(name):
                def run(e):
                    for waits, fns, sem, inc in self.q[name]:
                        for s, v in waits:
                            e.wait_ge(s, v)
                        ins = None
                        for f in fns:
                            ins = f(e)
                        if ins is not None and sem is not None:
                            ins.then_inc(sem, inc)
                return run
            block.tensor(runner("pe"))
            block.scalar(runner("act"))
            block.vector(runner("dve"))
            block.gpsimd(runner("pool"))
            block.sync(runner("sp"))


class Ring:
    def __init__(self, items):
        self.items = items
        self.i = 0

    def next(self):
        it = self.items[self.i % len(self.items)]
        self.i += 1
        return it


ARENA_BYTES = 184 * 1024


class Arena:
    def __init__(self, nc, es):
        self.t = es.enter_context(nc.sbuf_tensor("arena", [128, ARENA_BYTES // 2], BF16))
        self.off = 0

    def reset(self):
        self.off = 0

    def alloc(self, shape, dt, parts=128):
        n = 1
        for s in shape:
            n *= s
        four = dt in (F32, mybir.dt.int32, mybir.dt.uint32)
        nb = n * (4 if four else 2)
        nb_al = (nb + 63) // 64 * 64
        assert self.off + nb_al <= ARENA_BYTES, ("arena overflow", self.off, nb_al)
        e0 = self.off // 2
        ap = self.t[0:parts, e0:e0 + nb // 2]
        if four:
            ap = ap.bitcast(dt)
        self.off += nb_al
        if len(shape) == 1:
            return ap
        names = " ".join("d%d" % i for i in range(len(shape)))
        kw = {"d%d" % i: shape[i] for i in range(len(shape))}
        return ap.rearrange("p (%s) -> p %s" % (names, names), **kw)


class _StopBuild(Exception):
    pass


def build_program(dbg=False, stop=None, dumps=()):
    nc = bass.Bass("TRN2", target_bir_lowering=False)

    def din(name, shape, dt=F32):
        return nc.dram_tensor(name, list(shape), dt, kind="ExternalInput").ap()

    def dscr(name, shape, dt=BF16):
        return nc.dram_tensor(name, list(shape), dt).ap()

    x_in = din("x", [T, D])
    ctx_in = din("ctx", [TC, D])
    cvec = din("cvec", [2, D])
    norm1_g = din("norm1_g", [2, D])
    norm2_g = din("norm2_g", [2, D])
    w_ada = din("w_ada", [2, D, 6 * D])
    b_ada = din("b_ada", [2, 6 * D])
    w_in = din("w_in", [2, D, PW])
    conv_w = din("conv_w", [2, 3, 256])
    w_fourier = din("w_fourier", [2, 256, D])
    w_na = din("w_na", [2, 512, D])
    w_conv_out = din("w_conv_out", [2, 256, D])
    w_out = din("w_out", [2, D, D])
    ffn_wg = din("ffn_w_gate", [1, D, 2816])
    ffn_wu = din("ffn_w_up", [1, D, 2816])
    ffn_wd = din("ffn_w_down", [1, 2816, D])
    moe_router = din("moe_router", [1, D, 8])
    moe_wg = din("moe_w_gate", [1, 8, D, 3584])
    moe_wu = din("moe_w_up", [1, 8, D, 3584])
    moe_wd = din("moe_w_down", [1, 8, 3584, D])
    final_g = din("final_g", [1, D])
    biasT = din("biasT", [5, 128, 8 * 5 * 128], BF16)
    biasT1 = din("biasT1", [5, 128, 8 * 6 * 128], BF16)
    edgemask = din("edgemask", [1, 2])
    dftA = din("dftA", [128, 64 * 2 * 128], BF16)
    dft64 = din("dft64", [64, 3 * 64], BF16)
    dftc = din("dftc", [256, 2 * 256], BF16)
    c64bd = din("c64bd", [256, 2 * 256], BF16)
    y_out = nc.dram_tensor("y", [HALF, D], F32, kind="ExternalOutput").ap()

    xa = dscr("xa", [TT, D], F32)
    xb = dscr("xb", [XB_ROWS, D], F32)
    Ud = dscr("Ud", [TT, 256])
    qTd = dscr("qTd", [512, TT])
    kTd = dscr("kTd", [512, TT])
    vd = dscr("vd", [TT, 520])
    pTd = dscr("pTd", [256, TT])
    bTd = dscr("bTd", [256, TT])
    gTd = dscr("gTd", [3072, TT])
    Gd = dscr("Gd", [64, 128, 512])
    XTd = dscr("XTd", [512, TT])
    attnTd = dscr("attnTd", [512, TT])
    convTd = dscr("convTd", [256, TT])
    h2Td = dscr("h2Td", [D, TT])
    xloc = dscr("xloc", [LT, D], F32)
    XTloc = dscr("XTloc", [512, HALF])
    modd = dscr("modd", [2, 6 * D], F32)
    wrT = dscr("wrT", [8, D], F32)
    gatesd = dscr("gatesd", [T, 8], F32)
    fwg = dscr("fwg", [11, 128, 8 * 256])
    fwu = dscr("fwu", [11, 128, 8 * 256])
    fwd = dscr("fwd", [2816, D])
    mwgu = [dscr("mwgu%d" % q, [8 * D, 1792]) for q in range(4)]
    NGRP = 23
    Hs = dscr("Hs", [NGRP * 512, D])
    Ys = dscr("Ys", [NGRP * 512, D], F32)
    h2tok = dscr("h2tok", [HALF, D])
    mwd = dscr("mwd", [8, 3584, D])
    dbgs = {}
    scr_by_name = dict(xa=xa, xb=xb, Ud=Ud, qTd=qTd, kTd=kTd, vd=vd, pTd=pTd, bTd=bTd, gTd=gTd, Gd=Gd, XTd=XTd, attnTd=attnTd,
                       convTd=convTd, h2Td=h2Td, modd=modd, wrT=wrT, gatesd=gatesd, fwd=fwd, mwd=mwd, Hs=Hs, Ys=Ys, h2tok=h2tok)
    dump_out = {}
    for dn in dumps:
        src_ = scr_by_name[dn]
        dump_out[dn] = nc.dram_tensor("dump_" + dn, list(src_.shape), src_.dtype, kind="ExternalOutput").ap()
    if dbg:
        dbgs["xb1"] = nc.dram_tensor("dbg_xb1", [XB_ROWS, D], F32, kind="ExternalOutput").ap()
        dbgs["xa0"] = nc.dram_tensor("dbg_xa0", [TT, D], F32, kind="ExternalOutput").ap()

    with ExitStack() as es:
        S = Sched(nc, es)
        AR = Arena(nc, es)

        def sbt(name, shape, dt):
            return es.enter_context(nc.sbuf_tensor(name, shape, dt))

        identf = sbt("identf", [128, 128], F32)
        ident = sbt("ident", [128, 128], BF16)
        epsb = sbt("epsb", [128, 1], F32)
        scT = sbt("scT", [128, 8, 2], BF16)
        r_const = Res("const")
        Gall = sbt("Gall", [128, 32, 8], F32)
        r_Gall = Res("Gall", multi=True)
        I32 = mybir.dt.int32
        U32 = mybir.dt.uint32
        d12i = sbt("d12i", [128, 2, 32], I32)
        g12 = sbt("g12", [128, 2, 32], F32)
        r_route = Res("route", multi=True)
        r_h2tok = Res("h2tok", multi=True)
        psf = [(es.enter_context(nc.psum_tensor("psf%d" % i, [128, 512], F32)), Res("psf%d" % i)) for i in range(6)]
        psb = [(es.enter_context(nc.psum_tensor("psb%d" % i, [128, 1024], BF16)), Res("psb%d" % i)) for i in range(2)]
        PF = Ring(psf)
        PB = Ring(psb)
        evac_rr = [0]

        def evac_eng():
            evac_rr[0] += 1
            return "act" if evac_rr[0] % 2 else "dve"


        def f_mm(o, l, r, st, sp):
            return lambda e: e.matmul(o, lhsT=l, rhs=r, start=st, stop=sp)

        def f_tr(o, i):
            return lambda e: e.transpose(o, i, ident[:])

        def f_act(o, i, func, **kw):
            return lambda e: e.activation(out=o, in_=i, func=func, **kw)

        def f_tt(o, a, b, op):
            return lambda e: e.tensor_tensor(out=o, in0=a, in1=b, op=op)

        def f_ts(o, a, s1, op0):
            return lambda e: e.tensor_scalar(out=o, in0=a, scalar1=s1, scalar2=None, op0=op0)

        def f_stt(o, a, sc, b, op0, op1, **kw):
            return lambda e: e.scalar_tensor_tensor(out=o, in0=a, scalar=sc, in1=b, op0=op0, op1=op1, **kw)

        def f_rec(o, i):
            return lambda e: e.reciprocal(out=o, in_=i)

        def f_ms(o, v):
            return lambda e: e.memset(o, v)

        def copy_op(eng, out, in_, reads, writes, scale=None):
            if eng == "act":
                if scale is None:
                    S.op("act", lambda e: e.copy(out=out, in_=in_), reads=reads, writes=writes)
                else:
                    S.op("act", lambda e: e.mul(out=out, in_=in_, mul=scale), reads=reads, writes=writes)
            else:
                if scale is None:
                    S.op("dve", lambda e: e.tensor_copy(out=out, in_=in_), reads=reads, writes=writes)
                else:
                    S.op("dve", lambda e: e.tensor_scalar(out=out, in0=in_, scalar1=scale, scalar2=None, op0=ALU.mult), reads=reads, writes=writes)

        S.op("pool", lambda e: e.memset(identf[:], 0.0), writes=[r_const])
        S.op("pool", lambda e: e.affine_select(out=identf[:], in_=identf[:], pattern=[[-1, 128]], compare_op=ALU.not_equal,
                                               fill=1.0, base=0, channel_multiplier=1), reads=[r_const], writes=[r_const])
        S.op("pool", lambda e: e.memset(epsb[:], 1e-6), writes=[r_const])
        S.op("dve", lambda e: e.tensor_copy(out=ident[:], in_=identf[:]), reads=[r_const], writes=[r_const])
        r_sc = Res("sc")
        cv = sbt("cv", [2, D], F32)
        cvs = sbt("cvs", [2, D], F32)
        S.dma("sp", cv[:], cvec[:, :], writes=[r_sc])
        S.op("act", f_act(cvs[:], cv[:], AF.Silu), reads=[r_sc], writes=[r_sc])
        pb0, r_pb0 = psf[0]
        S.op("pe", [(lambda o, i: (lambda e: e.transpose(o, i, identf[0:2, 0:2])))(pb0[:, k * 2:(k + 1) * 2], cvs[0:2, k * 128:(k + 1) * 128]) for k in range(8)],
             reads=[r_sc, r_const], writes=[r_pb0])
        S.op("dve", lambda e: e.tensor_copy(out=scT[:], in_=pb0[:, 0:16].rearrange("p (k r) -> p k r", r=2)), reads=[r_pb0], writes=[r_const])

        r_xa = Res("xa", multi=True)
        r_xb = Res("xb", multi=True)
        r_xloc = Res("xloc", multi=True)
        r_XTloc = Res("XTloc", multi=True)
        for i in range(8):
            S.dma("sp", xb[PADR + i * 1024:PADR + (i + 1) * 1024, :], x_in[i * 1024:(i + 1) * 1024, :], writes=[r_xb])
        S.dma("sp", xb[CT0:CT0 + TC, :], ctx_in[:, :], writes=[r_xb])
        zt = sbt("zt", [128, D], F32)
        r_zt = Res("zt")
        S.op("pool", f_ms(zt[:], 0.0), writes=[r_zt])
        for i in range(4):
            S.dma("sp", xb[i * 128:(i + 1) * 128, :], zt[:], reads=[r_zt], writes=[r_xb])
            S.dma("sp", xb[PADR + T + i * 128:PADR + T + (i + 1) * 128, :], zt[:], reads=[r_zt], writes=[r_xb])

        _offc = {}

        def offv(e):
            if "v" not in _offc:
                _offc["v"] = e.snap((e.partition_id() % 2) * HALF, min_val=0, max_val=HALF)
            return _offc["v"]

        def dyn_rows(dst, src, row0, nrows):
            sl = src[row0:row0 + HALF + nrows, :]
            return lambda e: e.dma_start(out=dst, in_=sl[bass.ds(offv(e), nrows), :])

        def dyn_cols(dst, src, col0, ncols, pat, **kw):
            sl = src[:, col0:col0 + HALF + ncols]
            return lambda e: e.dma_start(out=dst, in_=sl[:, bass.ds(offv(e), ncols)].rearrange(pat, **kw))
        r_fw = Res("fw", multi=True)
        r_mw = Res("mw", multi=True)

        def bg_casts():
            for b_ in range(11):
                S.dma("pool", fwg[b_].rearrange("p (k f) -> p k f", k=8), ffn_wg[0, :, b_ * 256:(b_ + 1) * 256].rearrange("(k p) f -> p k f", p=128), writes=[r_fw])
                S.dma("pool", fwu[b_].rearrange("p (k f) -> p k f", k=8), ffn_wu[0, :, b_ * 256:(b_ + 1) * 256].rearrange("(k p) f -> p k f", p=128), writes=[r_fw])
            for k in range(11):
                S.dma("pool", fwd[k * 256:(k + 1) * 256, :], ffn_wd[0, k * 256:(k + 1) * 256, :], writes=[r_fw])
            for e_ in range(8):
                for k in range(2):
                    for q in range(4):
                        r0 = e_ * D + k * 512
                        S.dma("pool", mwgu[q][r0:r0 + 512, 0:896], moe_wg[0, e_, k * 512:(k + 1) * 512, q * 896:(q + 1) * 896], writes=[r_mw])
                        S.dma("pool", mwgu[q][r0:r0 + 512, 896:1792], moe_wu[0, e_, k * 512:(k + 1) * 512, q * 896:(q + 1) * 896], writes=[r_mw])
                for k in range(14):
                    S.dma("pool", mwd[e_, k * 256:(k + 1) * 256, :], moe_wd[0, e_, k * 256:(k + 1) * 256, :], writes=[r_mw])

        R = {n: Res(n, multi=True) for n in ("U", "qT", "kT", "v", "pT", "bT", "gT", "Gd", "XT", "attnT", "convT", "h2T", "modd", "gates")}

        SEQS = [(0, T, 0), (T, TC, 1)]

        def norm_tile(xt, r_x, gm, sh, r_mod, out_bf, r_out, tmp, r_tmp, ss, r_ss, out_f32=None):
            S.op("act", f_act(tmp, xt, AF.Square, accum_out=ss), reads=[r_x], writes=[r_tmp, r_ss])
            S.op("act", f_act(ss, ss, AF.Sqrt, bias=epsb[:], scale=1.0 / D), reads=[r_ss, r_const], writes=[r_ss])
            S.op("dve", f_rec(ss, ss), reads=[r_ss], writes=[r_ss])
            S.op("dve", f_stt(tmp, xt, ss, gm, ALU.mult, ALU.mult), reads=[r_x, r_ss, r_mod], writes=[r_tmp])
            if out_f32 is None:
                S.op("pool", f_tt(out_bf, tmp, sh, ALU.add), reads=[r_tmp, r_mod], writes=[r_out])
            else:
                S.op("pool", f_tt(out_f32, tmp, sh, ALU.add), reads=[r_tmp, r_mod], writes=[r_out])
                S.op("act", (lambda o, i: (lambda e: e.copy(out=o, in_=i)))(out_bf, out_f32), reads=[r_out], writes=[r_out])

        def transpose_to(hb, r_hb, dst, r_dst, nchunk):
            pt, r_pt = PB.next()
            S.op("pe", [f_tr(pt[:, k * 128:(k + 1) * 128], hb[:, k * 128:(k + 1) * 128]) for k in range(nchunk)],
                 reads=[r_hb, r_const], writes=[r_pt])
            copy_op(evac_eng(), dst, pt[:, 0:nchunk * 128].rearrange("p (k t) -> p k t", t=128), [r_pt], [r_dst])

        try:
          for l in range(2):
            last = (l == 1)
            if stop == (l, "P0"):
                raise _StopBuild()
            S.barrier(); AR.reset()
            wada = AR.alloc([8, 6 * D], BF16)
            r_w = Res("wada", multi=True)
            for k in range(8):
                S.dma("pool", wada[:, k, :], w_ada[l, k * 128:(k + 1) * 128, :], writes=[r_w])
            bada = AR.alloc([6 * D], F32, parts=2)
            r_b = Res("bada")
            S.dma("sp", bada, b_ada[l:l + 1, :].partition_broadcast(2), writes=[r_b])
            modt = AR.alloc([6 * D], F32, parts=2)
            r_mt = Res("modt", multi=True)
            for j in range(12):
                pb, r_pb = PF.next()
                S.op("pe", [f_mm(pb[0:2, :], scT[:, k, :], wada[:, k, j * 512:(j + 1) * 512], k == 0, k == 7) for k in range(8)],
                     reads=[r_w, r_const], writes=[r_pb])
                S.op("dve", f_tt(modt[:, j * 512:(j + 1) * 512], pb[0:2, :], bada[:, j * 512:(j + 1) * 512], ALU.add),
                     reads=[r_pb, r_b], writes=[r_mt])
            S.dma("sp", modd[:, :], modt, reads=[r_mt], writes=[R["modd"]])

            if stop == (l, "P1"):
                raise _StopBuild()
            S.barrier(); AR.reset()
            win = AR.alloc([8, PW], BF16)
            r_win = Res("win", multi=True)
            for k in range(8):
                S.dma("pool", win[:, k, :], w_in[l, k * 128:(k + 1) * 128, :], writes=[r_win])
            if l == 0:
                bg_casts()
            g1 = AR.alloc([D], F32)
            r_g = Res("g1")
            S.dma("sp", g1, norm1_g[l:l + 1, :].partition_broadcast(128), writes=[r_g])
            gms, shs, r_mods = [], [], []
            for (t0, tl, row) in SEQS:
                gm = AR.alloc([D], F32); sh = AR.alloc([D], F32); r_m = Res("mod%d" % row, multi=True)
                S.dma("sp", sh, modd[row:row + 1, 0:D].partition_broadcast(128), reads=[R["modd"]], writes=[r_m])
                S.dma("sp", gm, modd[row:row + 1, D:2 * D].partition_broadcast(128), reads=[R["modd"]], writes=[r_m])
                S.op("dve", f_stt(gm, gm, 1.0, g1, ALU.add, ALU.mult), reads=[r_m, r_g], writes=[r_m])
                gms.append(gm); shs.append(sh); r_mods.append(r_m)
            xts = Ring([(AR.alloc([D], F32), Res("xt%d" % i)) for i in range(2)])
            tmps = Ring([(AR.alloc([D], F32), Res("tmpf%d" % i)) for i in range(2)])
            sss = Ring([(AR.alloc([1], F32), Res("ss%d" % i)) for i in range(2)])
            hbs = Ring([(AR.alloc([D], BF16), Res("hb%d" % i)) for i in range(8)])
            hTs = Ring([(AR.alloc([8, 512], BF16), Res("hT%d" % i, multi=True)) for i in range(2)])
            stg = Ring([(AR.alloc([512], BF16), Res("stg%d" % i)) for i in range(6)])
            ustg = Ring([(AR.alloc([2, 512], BF16), Res("ustg%d" % i, multi=True)) for i in range(2)])
            vstg = Ring([(AR.alloc([8, 65], BF16), Res("vstg%d" % i)) for i in range(2)])
            for (vt, r_vt) in vstg.items:
                S.op("pool", f_ms(vt, 1.0), writes=[r_vt])
            def p1_A1(xload, gs, si, col0, do_u, do_rest):
                out = []
                for tt in range(gs // 128):
                    xt, r_xt = xts.next()
                    xload(tt, xt, r_xt)
                    hb, r_hb = hbs.next()
                    tmpf, r_tmpf = tmps.next(); ssq, r_ssq = sss.next()
                    norm_tile(xt, r_xt, gms[si], shs[si], r_mods[si], hb, r_hb, tmpf, r_tmpf, ssq, r_ssq)
                    out.append((hb, r_hb))
                return out

            def p1_A2(hbl):
                hT, r_hT = hTs.next()
                for tt, (hb, r_hb) in enumerate(hbl):
                    transpose_to(hb, r_hb, hT[:, :, tt * 128:(tt + 1) * 128], r_hT, 8)
                return (hT, r_hT)

            def p1_B(hTt, xload, gs, si, col0, do_u, do_rest):
                hT, r_hT = hTt
                g0 = col0
                if do_rest:
                    ust, r_ust = ustg.next()
                    for ch in range(44):
                        c0 = ch * 128
                        if ch in (0, 1, 10, 11, 12, 13):
                            continue
                        pb, r_pb = PF.next()
                        S.op("pe", [f_mm(pb[:, 0:gs], win[:, k, c0:c0 + 128], hT[:, k, 0:gs], k == 0, k == 7) for k in range(8)],
                             reads=[r_win, r_hT], writes=[r_pb])
                        if 14 <= ch <= 15:
                            copy_op(evac_eng(), ust[:, ch - 14, 0:gs], pb[:, 0:gs], [r_pb], [r_ust])
                            continue
                        st, r_st = stg.next()
                        if 2 <= ch <= 5:
                            copy_op(evac_eng(), st[:, 0:gs], pb[:, 0:gs], [r_pb], [r_st], scale=0.125)
                            dst, rd = qTd[(ch - 2) * 128:(ch - 1) * 128, g0:g0 + gs], R["qT"]
                        elif 6 <= ch <= 9:
                            copy_op(evac_eng(), st[:, 0:gs], pb[:, 0:gs], [r_pb], [r_st])
                            dst, rd = kTd[(ch - 6) * 128:(ch - 5) * 128, g0:g0 + gs], R["kT"]
                        elif 16 <= ch <= 17:
                            copy_op(evac_eng(), st[:, 0:gs], pb[:, 0:gs], [r_pb], [r_st])
                            dst, rd = bTd[(ch - 16) * 128:(ch - 15) * 128, g0:g0 + gs], R["bT"]
                        elif 18 <= ch <= 19:
                            S.op("dve", f_tt(st[:, 0:gs], pb[:, 0:gs], ust[:, ch - 18, 0:gs], ALU.mult), reads=[r_pb, r_ust], writes=[r_st])
                            dst, rd = pTd[(ch - 18) * 128:(ch - 17) * 128, g0:g0 + gs], R["pT"]
                        else:
                            S.op("act", f_act(st[:, 0:gs], pb[:, 0:gs], AF.Sigmoid), reads=[r_pb], writes=[r_st])
                            dst, rd = gTd[(ch - 20) * 128:(ch - 19) * 128, g0:g0 + gs], R["gT"]
                        S.dma("sp", dst, st[:, 0:gs], reads=[r_st], writes=[rd])
                for tt in range(gs // 128):
                    tok0 = g0 + tt * 128
                    if do_u:
                        pb, r_pb = PF.next()
                        S.op("pe", [f_mm(pb[:, 0:256], hT[:, k, tt * 128:(tt + 1) * 128], win[:, k, 0:256], k == 0, k == 7) for k in range(8)],
                             reads=[r_win, r_hT], writes=[r_pb])
                        st, r_st = stg.next()
                        copy_op(evac_eng(), st[:, 0:256], pb[:, 0:256], [r_pb], [r_st])
                        S.dma("sp", Ud[tok0:tok0 + 128, :], st[:, 0:256], reads=[r_st], writes=[R["U"]])
                    if do_rest:
                        pb, r_pb = PF.next()
                        S.op("pe", [f_mm(pb[:, :], hT[:, k, tt * 128:(tt + 1) * 128], win[:, k, 1280:1792], k == 0, k == 7) for k in range(8)],
                             reads=[r_win, r_hT], writes=[r_pb])
                        vt, r_vt = vstg.next()
                        copy_op(evac_eng(), vt[:, :, 0:64], pb[:, :].rearrange("p (h d) -> p h d", d=64), [r_pb], [r_vt])
                        S.dma("sp", vd[tok0:tok0 + 128, :], vt.rearrange("p h d -> p (h d)"), reads=[r_vt], writes=[R["v"]])

            def static_xload(g0):
                def f(tt, xt, r_xt):
                    r0 = xbrow(g0 + tt * 128)
                    S.dma("sp", xt, xb[r0:r0 + 128, :], reads=[r_xb], writes=[r_xt])
                return f

            def dyn_xload(row0):
                def f(tt, xt, r_xt):
                    S.dma("sp", xt, xloc[row0 + tt * 128:row0 + (tt + 1) * 128, :], reads=[r_xloc], writes=[r_xt])
                return f

            if last:
                for i5 in range(LT // 1024):
                    S.dma_fn("sp", dyn_rows(xloc[i5 * 1024:(i5 + 1) * 1024, :], xb, i5 * 1024, 1024), reads=[r_xb], writes=[r_xloc])
            if not last:
                glist = [(static_xload(g0), 512, 0, g0, True, True) for g0 in range(0, T, 512)]
                glist.append((static_xload(T), TC, 1, T, True, True))
            else:
                glist = [(static_xload(g0), 512, 0, g0, True, False) for g0 in range(0, T, 512)]
                glist.append((static_xload(T), TC, 1, T, False, True))
                glist += [(dyn_xload(j * 512), 512, 0, j * 512, False, True) for j in range(LT // 512)]
            ng_ = len(glist)
            hbl = {0: p1_A1(*glist[0])}
            hTl = {0: p1_A2(hbl.pop(0))}
            if ng_ > 1:
                hbl[1] = p1_A1(*glist[1])
                hTl[1] = p1_A2(hbl.pop(1))
            for gi in range(ng_):
                if gi + 2 < ng_:
                    hbl[gi + 2] = p1_A1(*glist[gi + 2])
                p1_B(hTl.pop(gi), *glist[gi])
                if gi + 2 < ng_:
                    hTl[gi + 2] = p1_A2(hbl.pop(gi + 2))

            if stop == (l, "P2"):
                raise _StopBuild()
            S.barrier(); AR.reset()
            t64 = AR.alloc([3, 64], BF16, parts=64); r_t64 = Res("t64")
            S.dma("sp", t64, dft64.rearrange("p (a b) -> p a b", b=64), writes=[r_t64])
            ubs = Ring([(AR.alloc([8, 256], BF16), Res("ub%d" % i)) for i in range(2)])
            tbs = Ring([(AR.alloc([8, 2, 128], BF16), Res("tb%d" % i)) for i in range(2)])
            gss = Ring([(AR.alloc([8, 512], BF16), Res("gs%d" % i, multi=True)) for i in range(2)])
            Uv = Ud[0:T, :].rearrange("(a b) c -> a b c", b=64)
            dAv = dftA.rearrange("p (a r k) -> p a r k", r=2, k=128)
            for lb in range(8):
                ub, r_ub = ubs.next(); tb, r_tb = tbs.next(); gsb, r_gs = gss.next()
                S.dma("sp", ub, Uv[:, lb * 8:(lb + 1) * 8, :], reads=[R["U"]], writes=[r_ub])
                S.dma("sp", tb, dAv[:, lb * 8:(lb + 1) * 8, :, :], writes=[r_tb])
                for i in range(8):
                    pb, r_pb = PF.next()
                    S.op("pe", [f_mm(pb[:, pr * 256:(pr + 1) * 256], tb[:, i, pr, :], ub[:, i, :], True, True) for pr in range(2)],
                         reads=[r_ub, r_tb], writes=[r_pb])
                    copy_op(evac_eng(), gsb[:, i, :], pb[:, :], [r_pb], [r_gs])
                S.dma("sp", Gd[lb * 8:(lb + 1) * 8, :, :].rearrange("l k c -> k l c"), gsb, reads=[r_gs], writes=[R["Gd"]])
            xts4 = [(AR.alloc([64, 128], BF16), Res("xts%d" % i, multi=True)) for i in range(4)]
            gbs = Ring([(AR.alloc([8, 512], BF16, parts=64), Res("gb%d" % i)) for i in range(2)])
            for kb in range(16):
                gb, r_gb = gbs.next()
                S.dma("sp", gb, Gd[:, kb * 8:(kb + 1) * 8, :], reads=[R["Gd"]], writes=[r_gb])
                for ri in range(2):
                    for chalf in range(2):
                        pb, r_pb = PF.next()
                        fns = []
                        c0 = chalf * 128
                        for i in range(8):
                            o = pb[:, i * 64:(i + 1) * 64]
                            if ri == 0:
                                fns.append(f_mm(o, gb[:, i, c0:c0 + 128], t64[:, 0, :], True, False))
                                fns.append(f_mm(o, gb[:, i, 256 + c0:256 + c0 + 128], t64[:, 1, :], False, True))
                            else:
                                fns.append(f_mm(o, gb[:, i, 256 + c0:256 + c0 + 128], t64[:, 0, :], True, False))
                                fns.append(f_mm(o, gb[:, i, c0:c0 + 128], t64[:, 2, :], False, True))
                        S.op("pe", fns, reads=[r_gb, r_t64], writes=[r_pb])
                        xt4, r_x4 = xts4[ri * 2 + chalf]
                        copy_op(evac_eng(), xt4[:, :, kb * 8:(kb + 1) * 8].rearrange("p a b -> p b a"), pb[:, :].rearrange("p (b a) -> p b a", a=64), [r_pb], [r_x4])
            for i4 in range(4):
                xt4, r_x4 = xts4[i4]
                S.dma("sp", XTd[i4 * 128:(i4 + 1) * 128, 0:T], xt4.rearrange("p a b -> p (a b)"), reads=[r_x4], writes=[R["XT"]])
            uc = AR.alloc([2, 256], BF16); r_uc = Res("uc")
            S.dma("sp", uc, Ud[T:TT, :].rearrange("(a p) c -> p a c", p=128), reads=[R["U"]], writes=[r_uc])
            tcx = AR.alloc([2, 2, 256], BF16); r_tc = Res("tcx")
            S.dma("sp", tcx, dftc.rearrange("(a p) (r k) -> p a r k", p=128, r=2), writes=[r_tc])
            xcs = AR.alloc([4, 256], BF16); r_xc = Res("xcs", multi=True)
            for ri in range(2):
                for chalf in range(2):
                    pb, r_pb = PF.next()
                    S.op("pe", [f_mm(pb[:, 0:256], uc[:, a, chalf * 128:(chalf + 1) * 128], tcx[:, a, ri, :], a == 0, a == 1) for a in range(2)],
                         reads=[r_uc, r_tc], writes=[r_pb])
                    copy_op(evac_eng(), xcs[:, ri * 2 + chalf, :], pb[:, 0:256], [r_pb], [r_xc])
            S.dma("sp", XTd[:, T:TT].rearrange("(j p) t -> p j t", p=128), xcs, reads=[r_xc], writes=[R["XT"]])
            if last:
                for i4 in range(4):
                    S.dma_fn("sp", (lambda d_, sl_: (lambda e: e.dma_start(out=d_, in_=sl_[:, bass.ds(offv(e), HALF)])))(XTloc[i4 * 128:(i4 + 1) * 128, :], XTd[i4 * 128:(i4 + 1) * 128, 0:T]),
                             reads=[R["XT"]], writes=[r_XTloc])

            if stop == (l, "P3"):
                raise _StopBuild()
            S.barrier(); AR.reset()
            NW = 6 if last else 5
            bias = AR.alloc([5, 8, NW, 128], BF16); r_bias = Res("bias", multi=True)
            bsrc = biasT1 if last else biasT
            for v_ in range(5):
                S.dma("sp", bias[:, v_], bsrc[v_].rearrange("p (h j q) -> p h j q", h=8, j=NW), writes=[r_bias])
            for v_ in range(5):
                S.op("act", f_act(bias[:, v_].rearrange("p h j q -> p (h j q)"), bias[:, v_].rearrange("p h j q -> p (h j q)"), AF.Exp), reads=[r_bias], writes=[r_bias])
            Kc = AR.alloc([4, 256], BF16); Vc = AR.alloc([2, 520], BF16); r_kvc = Res("kvc", multi=True)
            S.dma("sp", Kc, kTd[:, T:TT].rearrange("(j p) t -> p j t", p=128), reads=[R["kT"]], writes=[r_kvc])
            S.dma("sp", Vc, vd[T:TT, :].rearrange("(j p) c -> p j c", p=128), reads=[R["v"]], writes=[r_kvc])
            Qs = Ring([(AR.alloc([4, 512], BF16), Res("Q%d" % i)) for i in range(2)])
            Kws = Ring([(AR.alloc([4, NW * 128], BF16), Res("Kw%d" % i)) for i in range(3)])
            Vws = Ring([(AR.alloc([NW, 520], BF16), Res("Vw%d" % i)) for i in range(3)])
            pTs = Ring([(AR.alloc([(NW + 2) * 128], BF16), Res("pT%d" % i, multi=True)) for i in range(4)])
            recs = Ring([(AR.alloc([8], F32), Res("rec%d" % i, multi=True)) for i in range(2)])
            atts = Ring([(AR.alloc([8, 64], BF16), Res("att%d" % i, multi=True)) for i in range(2)])
            aTst = Ring([(AR.alloc([4, 512], BF16), Res("aTst%d" % i, multi=True)) for i in range(2)])
            PS4 = Ring(psf[0:4])
            if not last:
                tiles = [(128 * m, 64 * min(max(2 * m - 4, 0), 118), {0: 1, 1: 2, 62: 3, 63: 4}.get(m, 0), True) for m in range(64)]
                tiles += [(T, 0, 0, False), (T + 128, 0, 0, False)]
            else:
                tiles = [(PADR + 128 * m, PADR + 128 * m - (384 if m == 31 else 256), {0: 1, 1: 2, 30: 3, 31: 4}.get(m, 0), True) for m in range(32)]
            pO = [psf[4], psf[5]]
            tstate = {"Q": None, "r_Q": None, "aT": None, "r_aT": None}

            def att_pre(ti):
                tok0, kcol, var, lat = tiles[ti]
                c = {"tok0": tok0, "var": var, "lat": lat, "ti": ti}
                if lat:
                    if ti % 4 == 0:
                        tstate["Q"], tstate["r_Q"] = Qs.next()
                        S.dma("sp", tstate["Q"], qTd[:, tok0:tok0 + 512].rearrange("(j p) t -> p j t", p=128), reads=[R["qT"]], writes=[tstate["r_Q"]])
                        tstate["aT"], tstate["r_aT"] = aTst.next()
                    c["qo"] = (ti % 4) * 128
                    c["Kw"], c["r_Kw"] = Kws.next(); c["Vw"], c["r_Vw"] = Vws.next()
                    S.dma("sp", c["Kw"], kTd[:, kcol:kcol + NW * 128].rearrange("(j p) t -> p j t", p=128), reads=[R["kT"]], writes=[c["r_Kw"]])
                    S.dma("sp", c["Vw"], vd[kcol:kcol + NW * 128, :].rearrange("(j p) c -> p j c", p=128), reads=[R["v"]], writes=[c["r_Vw"]])
                    c["nw"] = NW
                else:
                    if tok0 == T:
                        tstate["Q"], tstate["r_Q"] = Qs.next()
                        S.dma("sp", tstate["Q"][:, :, 0:256], qTd[:, T:TT].rearrange("(j p) t -> p j t", p=128), reads=[R["qT"]], writes=[tstate["r_Q"]])
                        tstate["aT"], tstate["r_aT"] = aTst.next()
                    c["qo"] = tok0 - T
                    c["nw"] = 0
                c["Q"], c["r_Q"], c["aT"], c["r_aT"] = tstate["Q"], tstate["r_Q"], tstate["aT"], tstate["r_aT"]
                c["nk"] = c["nw"] + 2
                return c

            def att_S(c, h):
                nw, nk, lat, Q, qo = c["nw"], c["nk"], c["lat"], c["Q"], c["qo"]
                hp, po = h // 2, (h % 2) * 64
                nbk = (nk + 3) // 4
                pS = [PS4.next() for _ in range(nbk)]
                fns = []
                for j in range(nk):
                    o = pS[j // 4][0][:, (j % 4) * 128:(j % 4 + 1) * 128]
                    if j < nw:
                        fns.append(f_mm(o, c["Kw"][po:po + 64, hp, j * 128:(j + 1) * 128], Q[po:po + 64, hp, qo:qo + 128], True, True))
                    else:
                        jj = j - nw
                        fns.append(f_mm(o, Kc[po:po + 64, hp, jj * 128:(jj + 1) * 128], Q[po:po + 64, hp, qo:qo + 128], True, True))
                S.op("pe", fns, reads=[c["r_Q"], r_kvc] + ([c["r_Kw"]] if lat else []), writes=[p_[1] for p_ in pS])
                pT, r_pT = pTs.next()
                for bk in range(nbk):
                    n_ = min(4, nk - 4 * bk) * 128
                    S.op("act", f_act(pT[:, bk * 512:bk * 512 + n_], pS[bk][0][:, 0:n_], AF.Exp), reads=[pS[bk][1]], writes=[r_pT])
                if lat:
                    S.op("dve", f_tt(pT[:, 0:nw * 128], pT[:, 0:nw * 128], bias[:, c["var"], h].rearrange("p j q -> p (j q)"), ALU.mult), reads=[r_pT, r_bias], writes=[r_pT])
                return (pT, r_pT)

            def att_PV(c, h, pTt):
                pT, r_pT = pTt
                nw, nk, lat = c["nw"], c["nk"], c["lat"]
                po_t, r_po = pO[h // 4]
                o = po_t[:, (h % 4) * 65:(h % 4 + 1) * 65]
                fns = []
                for j in range(nk):
                    if j < nw:
                        fns.append(f_mm(o, pT[:, j * 128:(j + 1) * 128], c["Vw"][:, j, h * 65:(h + 1) * 65], j == 0, False))
                    else:
                        jj = j - nw
                        fns.append(f_mm(o, pT[:, j * 128:(j + 1) * 128], Vc[:, jj, h * 65:(h + 1) * 65], j == 0, j == nk - 1))
                S.op("pe", fns, reads=[r_pT, r_kvc] + ([c["r_Vw"]] if lat else []), writes=[r_po])

            def att_norm(c):
                rec, r_rec = recs.next(); att, r_att = atts.next()
                for hh in range(2):
                    po_t, r_po = pO[hh]
                    pv = po_t[:, 0:260].rearrange("p (h d) -> p h d", d=65)
                    S.op("dve", f_rec(rec[:, hh * 4:(hh + 1) * 4], pv[:, :, 64]), reads=[r_po], writes=[r_rec])
                    S.op("dve", f_tt(att[:, hh * 4:(hh + 1) * 4, :], pv[:, :, 0:64], rec[:, hh * 4:(hh + 1) * 4].unsqueeze(2).to_broadcast([128, 4, 64]), ALU.mult),
                         reads=[r_po, r_rec], writes=[r_att])
                c["att"], c["r_att"] = att, r_att

            def att_post(c):
                tok0, lat, qo, aT, r_aT = c["tok0"], c["lat"], c["qo"], c["aT"], c["r_aT"]
                transpose_to(c["att"].rearrange("p h d -> p (h d)"), c["r_att"], aT[:, :, qo:qo + 128], r_aT, 4)
                if lat and c["ti"] % 4 == 3:
                    S.dma("sp", attnTd[:, tok0 - 384:tok0 + 128].rearrange("(j p) t -> p j t", p=128), aT, reads=[r_aT], writes=[R["attnT"]])
                elif (not lat) and tok0 == T + 128:
                    S.dma("sp", attnTd[:, T:TT].rearrange("(j p) t -> p j t", p=128), aT[:, :, 0:256], reads=[r_aT], writes=[R["attnT"]])

            items = [(ti, h) for ti in range(len(tiles)) for h in range(8)]
            ctxs = {0: att_pre(0)}
            pts = {0: att_S(ctxs[0], 0)}
            pending = None
            for i, (ti, h) in enumerate(items):
                if h == 2 and ti + 1 < len(tiles):
                    ctxs[ti + 1] = att_pre(ti + 1)
                if i + 1 < len(items):
                    nti, nh_ = items[i + 1]
                    if nh_ == 0 and nti not in ctxs:
                        ctxs[nti] = att_pre(nti)
                    pts[i + 1] = att_S(ctxs[nti], nh_)
                if pending is not None:
                    att_post(pending)
                    pending = None
                att_PV(ctxs[ti], h, pts.pop(i))
                if h == 7:
                    att_norm(ctxs[ti])
                    pending = ctxs.pop(ti)
            if pending is not None:
                att_post(pending)

            if stop == (l, "P4"):
                raise _StopBuild()
            S.barrier(); AR.reset()
            cw = AR.alloc([2, 3], F32); r_cw = Res("cw")
            cw3 = AR.alloc([256], F32, parts=3); r_cw3 = Res("cw3")
            S.dma("sp", cw3, conv_w[l], writes=[r_cw3])
            pbc, r_pbc = PF.next()
            S.op("pe", [(lambda o, i: (lambda e: e.transpose(o, i, identf[0:3, 0:3])))(pbc[:, c_ * 3:(c_ + 1) * 3], cw3[0:3, c_ * 128:(c_ + 1) * 128]) for c_ in range(2)],
                 reads=[r_cw3, r_const], writes=[r_pbc])
            S.op("dve", (lambda o, i: (lambda e: e.tensor_copy(out=o, in_=i)))(cw, pbc[:, 0:6].rearrange("p (c j) -> p c j", j=3)), reads=[r_pbc], writes=[r_cw])
            pins = Ring([(AR.alloc([2, 514], BF16), Res("pin%d" % i, multi=True)) for i in range(2)])
            bins = Ring([(AR.alloc([2, 512], BF16), Res("bin%d" % i)) for i in range(2)])
            cts = Ring([(AR.alloc([512], F32), Res("ct%d" % i)) for i in range(2)])
            cos_ = Ring([(AR.alloc([2, 512], BF16), Res("co%d" % i, multi=True)) for i in range(2)])
            if not last:
                cgroups = [(g0, 512, 0, T) for g0 in range(0, T, 512)] + [(T, TC, T, TT)]
            else:
                cgroups = [(512 * j, 512, 0, LT) for j in range(1, 9)]
                emk = AR.alloc([2], F32); r_emk = Res("emk")
                S.dma("sp", emk, edgemask[0:1, :].partition_broadcast(128), writes=[r_emk])
            for (g0, gs, t0, tend) in cgroups:
                if True:
                    tl = tend - t0
                    pin, r_pin = pins.next(); bi, r_bi = bins.next(); co, r_co = cos_.next()
                    lo = g0 - 1 if g0 > t0 else g0
                    hi = g0 + gs + 1 if g0 + gs < t0 + tl else g0 + gs
                    if lo == g0:
                        S.op("pool", f_ms(pin[:, :, 0:1], 0.0), writes=[r_pin])
                    if hi == g0 + gs:
                        S.op("pool", f_ms(pin[:, :, gs + 1:gs + 2], 0.0), writes=[r_pin])
                    S.dma("sp", pin[:, :, lo - g0 + 1:hi - g0 + 1], pTd[:, lo:hi].rearrange("(c p) t -> p c t", p=128), reads=[R["pT"]], writes=[r_pin])
                    S.dma("sp", bi[:, :, 0:gs], bTd[:, g0:g0 + gs].rearrange("(c p) t -> p c t", p=128), reads=[R["bT"]], writes=[r_bi])
                    if last and g0 == 512:
                        S.op("dve", f_ts(pin[:, :, 0:1], pin[:, :, 0:1], emk[:, 0:1], ALU.mult), reads=[r_pin, r_emk], writes=[r_pin])
                    if last and g0 == 512 * 8:
                        S.op("dve", f_ts(pin[:, :, gs + 1:gs + 2], pin[:, :, gs + 1:gs + 2], emk[:, 1:2], ALU.mult), reads=[r_pin, r_emk], writes=[r_pin])
                    for c in range(2):
                        ct, r_ct = cts.next()
                        S.op("dve", f_ts(ct[:, 0:gs], pin[:, c, 1:gs + 1], cw[:, c, 1:2], ALU.mult), reads=[r_pin, r_cw], writes=[r_ct])
                        S.op("dve", f_stt(ct[:, 0:gs], pin[:, c, 0:gs], cw[:, c, 0:1], ct[:, 0:gs], ALU.mult, ALU.add), reads=[r_pin, r_cw, r_ct], writes=[r_ct])
                        S.op("dve", f_stt(ct[:, 0:gs], pin[:, c, 2:gs + 2], cw[:, c, 2:3], ct[:, 0:gs], ALU.mult, ALU.add), reads=[r_pin, r_cw, r_ct], writes=[r_ct])
                        S.op("pool", f_tt(co[:, c, 0:gs], ct[:, 0:gs], bi[:, c, 0:gs], ALU.mult), reads=[r_ct, r_bi], writes=[r_co])
                    S.dma("sp", convTd[:, g0:g0 + gs].rearrange("(c p) t -> p c t", p=128), co[:, :, 0:gs], reads=[r_co], writes=[R["convT"]])

            if stop == (l, "P5a"):
                raise _StopBuild()
            S.barrier(); AR.reset()
            seqs5 = SEQS if not last else SEQS[:1]
            wf = AR.alloc([2, D], BF16); cbd = AR.alloc([2, 2, 256], BF16); r_wf = Res("wf", multi=True)
            S.dma("pool", wf, w_fourier[l].rearrange("(k p) n -> p k n", p=128), writes=[r_wf])
            S.dma("sp", cbd, c64bd.rearrange("(k p) (r c) -> p k r c", p=128, r=2), writes=[r_wf])
            wcs = AR.alloc([4, D], BF16); r_wcs = Res("wcs", multi=True)
            wna = AR.alloc([4, D], BF16); wco = AR.alloc([2, D], BF16); wo = AR.alloc([8, D], BF16); r_wm = Res("wm", multi=True)
            S.dma("pool", wna, w_na[l].rearrange("(k p) n -> p k n", p=128), writes=[r_wm])
            S.dma("pool", wco, w_conv_out[l].rearrange("(k p) n -> p k n", p=128), writes=[r_wm])
            S.dma("pool", wo, w_out[l].rearrange("(k p) n -> p k n", p=128), writes=[r_wm])
            for part in range(2):
                for chalf in range(2):
                    for nh in range(2):
                        pb, r_pb = PF.next()
                        S.op("pe", [f_mm(pb[:, :], cbd[:, k, part, chalf * 128:(chalf + 1) * 128], wf[:, k, nh * 512:(nh + 1) * 512], k == 0, k == 1) for k in range(2)],
                             reads=[r_wf], writes=[r_pb])
                        copy_op(evac_eng(), wcs[:, part * 2 + chalf, nh * 512:(nh + 1) * 512], pb[:, :], [r_pb], [r_wcs])
            g2 = AR.alloc([D], F32); r_g2 = Res("g2")
            S.dma("sp", g2, norm2_g[l:l + 1, :].partition_broadcast(128), writes=[r_g2])
            m5 = []
            for (t0, tl, row) in seqs5:
                gt = AR.alloc([D], F32); gm = AR.alloc([D], F32); sh = AR.alloc([D], F32); r_m = Res("m5_%d" % row, multi=True)
                S.dma("sp", gt, modd[row:row + 1, 2 * D:3 * D].partition_broadcast(128), reads=[R["modd"]], writes=[r_m])
                S.dma("sp", sh, modd[row:row + 1, 3 * D:4 * D].partition_broadcast(128), reads=[R["modd"]], writes=[r_m])
                S.dma("sp", gm, modd[row:row + 1, 4 * D:5 * D].partition_broadcast(128), reads=[R["modd"]], writes=[r_m])
                S.op("dve", f_stt(gm, gm, 1.0, g2, ALU.add, ALU.mult), reads=[r_m, r_g2], writes=[r_m])
                m5.append((gt, gm, sh, r_m))
            if last:
                wrb = AR.alloc([8, D], F32); r_wrb = Res("wrb", multi=True)
                wr_ = wrb[:, 0, 0:64].rearrange("p (k e) -> p k e", e=8); r_wr = Res("wr")
                wrTs = wrb[0:8, 1, :]; r_wrTs = Res("wrTs", multi=True)
                S.dma("sp", wr_, moe_router[0].rearrange("(p k) e -> p k e", k=8), writes=[r_wr])
                for hf_ in range(2):
                    pbr, r_pbr = PF.next()
                    S.op("pe", [(lambda o, i: (lambda e: e.transpose(o, i, identf[:, :])))(pbr[0:8, kk * 128:(kk + 1) * 128], wr_[:, hf_ * 4 + kk, :]) for kk in range(4)],
                         reads=[r_wr, r_const], writes=[r_pbr])
                    S.op("dve", (lambda o, i: (lambda e: e.tensor_copy(out=o, in_=i)))(
                        wrTs.rearrange("e (p k) -> e k p", k=8)[:, hf_ * 4:(hf_ + 1) * 4, :], pbr[0:8, :].rearrange("e (k p) -> e k p", p=128)),
                        reads=[r_pbr], writes=[r_wrTs])
                r_wrTd = Res("wrTd")
                S.dma("sp", wrT[:, :], wrTs, reads=[r_wrTs], writes=[r_wrTd])
                for e_ in range(8):
                    S.dma("sp", wrb[:, e_, :], wrT[e_:e_ + 1, :].partition_broadcast(128), reads=[r_wrTd], writes=[r_wrb, r_wr, r_wrTs])
            G5 = 256
            wos = []
            if not last:
                wo_c = AR.alloc([8, D], BF16); r_woc = Res("woc", multi=True)
                for k in range(8):
                    S.op("dve", f_tt(wo_c[:, k, :], wo[:, k, :], m5[1][0], ALU.mult), reads=[r_wm, m5[1][3]], writes=[r_woc])
            r_wos = Res("wos", multi=True)
            for k in range(8):
                S.op("dve", f_tt(wo[:, k, :], wo[:, k, :], m5[0][0], ALU.mult), reads=[r_wm, m5[0][3]] + ([r_woc] if not last else []), writes=[r_wos, r_wm])
            wos.append((wo, r_wos))
            if not last:
                wos.append((wo_c, r_woc))
            XTs = Ring([(AR.alloc([4, G5], BF16), Res("XTs%d" % i)) for i in range(2)])
            ATs = Ring([(AR.alloc([4, G5], BF16), Res("ATs%d" % i)) for i in range(2)])
            CTs = Ring([(AR.alloc([2, G5], BF16), Res("CTs%d" % i)) for i in range(2)])
            GTs = Ring([(AR.alloc([24, G5], BF16), Res("GTs%d" % i, multi=True)) for i in range(2)])
            sTs = Ring([(AR.alloc([8, G5], BF16), Res("sTs%d" % i, multi=True)) for i in range(2)])
            t1s = Ring([(AR.alloc([3, G5], F32), Res("t1s%d" % i, multi=True)) for i in range(2)])
            xrs = Ring([(AR.alloc([D], F32), Res("xr%d" % i)) for i in range(2)])
            xms = Ring([(AR.alloc([D], F32), Res("xm%d" % i, multi=True)) for i in range(2)])
            tmp5 = AR.alloc([D], F32); r_tmp5 = Res("tmp5")
            ss5 = AR.alloc([1], F32); r_ss5 = Res("ss5")
            hfs = Ring([(AR.alloc([D], F32), Res("hf%d" % i)) for i in range(2)])
            hb5 = Ring([AR.alloc([D], BF16) for i in range(2)])
            h2s = Ring([(AR.alloc([8, G5], BF16), Res("h2s%d" % i, multi=True)) for i in range(2)])
            lg = Ring([(AR.alloc([24], F32), Res("lg%d" % i)) for i in range(2)])
            if not last:
                groups5a = [(g0, G5, 0) for g0 in range(0, T, G5)] + [(T, TC, 1)]
                groups5 = [(g0, 512, 0) for g0 in range(0, T, 512)] + [(T, TC, 1)]
            else:
                groups5a = [(PADR + G5 * j, G5, 0) for j in range(HALF // G5)]
                groups5 = [(512 * j, 512, 0) for j in range(1, 9)]
            def p5_A(g0, gs, si):
                XT_, r_XT = XTs.next(); AT_, r_AT = ATs.next(); CT_, r_CT = CTs.next(); GT_, r_GT = GTs.next()
                if not last:
                    S.dma("sp", XT_[:, :, 0:gs], XTd[:, g0:g0 + gs].rearrange("(j p) t -> p j t", p=128), reads=[R["XT"]], writes=[r_XT])
                else:
                    S.dma("sp", XT_[:, :, 0:gs], XTloc[:, g0 - PADR:g0 - PADR + gs].rearrange("(j p) t -> p j t", p=128), reads=[r_XTloc], writes=[r_XT])
                S.dma("sp", AT_[:, :, 0:gs], attnTd[:, g0:g0 + gs].rearrange("(j p) t -> p j t", p=128), reads=[R["attnT"]], writes=[r_AT])
                S.dma("sp", CT_[:, :, 0:gs], convTd[:, g0:g0 + gs].rearrange("(j p) t -> p j t", p=128), reads=[R["convT"]], writes=[r_CT])
                for i3 in range(3):
                    S.dma("sp", GT_[:, i3 * 8:(i3 + 1) * 8, 0:gs], gTd[i3 * 1024:(i3 + 1) * 1024, g0:g0 + gs].rearrange("(j p) t -> p j t", p=128), reads=[R["gT"]], writes=[r_GT])
                sT, r_sT = sTs.next()
                for cc in range(8):
                    c0 = cc * 128
                    t1, r_t1 = t1s.next()
                    specs = [(wcs, r_wcs, XT_, r_XT, 4, 0), (wna, r_wm, AT_, r_AT, 4, 8), (wco, r_wm, CT_, r_CT, 2, 16)]
                    for bi_, (wt, r_wt, src, r_src, nk_, goff) in enumerate(specs):
                        pb, r_pb = PF.next()
                        S.op("pe", [f_mm(pb[:, 0:gs], wt[:, k, c0:c0 + 128], src[:, k, 0:gs], k == 0, k == nk_ - 1) for k in range(nk_)],
                             reads=[r_wt, r_src], writes=[r_pb])
                        S.op("dve", f_tt(t1[:, bi_, 0:gs], pb[:, 0:gs], GT_[:, goff + cc, 0:gs], ALU.mult), reads=[r_pb, r_GT], writes=[r_t1])
                    S.op("pool", f_tt(t1[:, 0, 0:gs], t1[:, 0, 0:gs], t1[:, 1, 0:gs], ALU.add), reads=[r_t1], writes=[r_t1])
                    S.op("pool", f_tt(sT[:, cc, 0:gs], t1[:, 0, 0:gs], t1[:, 2, 0:gs], ALU.add), reads=[r_t1], writes=[r_sT])
                return (sT, r_sT)

            def p5_B1(g0, gs, si, tt, sTt):
                sT, r_sT = sTt
                gt, gm, sh, r_m = m5[si]
                wo_s, r_wo_s = wos[si]
                tok0 = g0 + tt * 128
                xr, r_xr = xrs.next(); xm, r_xm = xms.next()
                if not last:
                    S.dma("sp", xr, xb[xbrow(tok0):xbrow(tok0) + 128, :], reads=[r_xb], writes=[r_xr])
                else:
                    S.dma("sp", xr, xloc[tok0:tok0 + 128, :], reads=[r_xloc], writes=[r_xr])
                for nh in range(2):
                    pb, r_pb = PF.next()
                    S.op("pe", [f_mm(pb[:, :], sT[:, k, tt * 128:(tt + 1) * 128], wo_s[:, k, nh * 512:(nh + 1) * 512], k == 0, k == 7) for k in range(8)],
                         reads=[r_sT, r_wo_s], writes=[r_pb])
                    S.op("dve", f_tt(xm[:, nh * 512:(nh + 1) * 512], pb[:, :], xr[:, nh * 512:(nh + 1) * 512], ALU.add), reads=[r_pb, r_xr], writes=[r_xm])
                S.dma("sp", xa[tok0:tok0 + 128, :], xm, reads=[r_xm], writes=[r_xa])
                hf, r_hf = hfs.next(); hb = hb5.next()
                norm_tile(xm, r_xm, gm, sh, r_m, hb, r_hf, tmp5, r_tmp5, ss5, r_ss5, out_f32=hf)
                return (tok0, hf, r_hf, hb)

            def p5_B2(tt, st_, h2t):
                tok0, hf, r_hf, hb = st_
                h2, r_h2 = h2t
                if not last:
                    transpose_to(hb, r_hf, h2[:, :, tt * 128:(tt + 1) * 128], r_h2, 8)
                else:
                    S.dma("sp", h2tok[tok0 - PADR:tok0 - PADR + 128, :], hb, reads=[r_hf], writes=[r_h2tok])
                    lgt, r_lg = lg.next()
                    S.op("pool", f_ms(lgt[:, 0:8], 0.0), writes=[r_lg])
                    for e_ in range(8):
                        S.op("dve", f_stt(tmp5, hf, 1.0, wrb[:, e_, :], ALU.mult, ALU.mult, accum_out=lgt[:, e_:e_ + 1]),
                             reads=[r_hf, r_wrb, r_lg], writes=[r_tmp5, r_lg])
                    S.op("dve", (lambda o, i: (lambda e: e.max(out=o, in_=i)))(lgt[:, 8:16], lgt[:, 0:8]), reads=[r_lg], writes=[r_lg])
                    S.op("dve", f_ts(lgt[:, 16:24], lgt[:, 0:8], lgt[:, 8:9], ALU.subtract), reads=[r_lg], writes=[r_lg])
                    S.op("act", f_act(lgt[:, 16:24], lgt[:, 16:24], AF.Exp), reads=[r_lg], writes=[r_lg])
                    S.op("dve", f_stt(lgt[:, 16:24], lgt[:, 0:8], lgt[:, 9:10], lgt[:, 16:24], ALU.is_ge, ALU.mult), reads=[r_lg], writes=[r_lg])
                    S.op("dve", (lambda o, i: (lambda e: e.reduce_sum(out=o, in_=i, axis=mybir.AxisListType.X)))(lgt[:, 8:9], lgt[:, 16:24]), reads=[r_lg], writes=[r_lg])
                    S.op("dve", f_rec(lgt[:, 8:9], lgt[:, 8:9]), reads=[r_lg], writes=[r_lg])
                    S.op("dve", f_ts(Gall[:, (tok0 - PADR) // 128, :], lgt[:, 16:24], lgt[:, 8:9], ALU.mult), reads=[r_lg], writes=[r_Gall])

            sT_next = p5_A(*groups5a[0])
            for gi, (g0, gs, si) in enumerate(groups5a):
                sT_cur = sT_next
                if gi + 1 < len(groups5a):
                    sT_next = p5_A(*groups5a[gi + 1])
                h2t = h2s.next()
                ntl = gs // 128
                sts = {}
                for tt in range(ntl):
                    sts[tt] = p5_B1(g0, gs, si, tt, sT_cur)
                    if tt >= 1:
                        p5_B2(tt - 1, sts.pop(tt - 1), h2t)
                p5_B2(ntl - 1, sts.pop(ntl - 1), h2t)
                if not last:
                    S.dma("sp", h2Td[:, g0:g0 + gs].rearrange("(k p) t -> p k t", p=128), h2t[0][:, :, 0:gs], reads=[h2t[1]], writes=[R["h2T"]])

            if stop == (l, "P5b"):
                raise _StopBuild()
            S.barrier(); AR.reset()
            if last:
                AXX = mybir.AxisListType.X
                def f_tsc(o, a, s1, op0):
                    return lambda e: e.tensor_scalar(out=o, in0=a, scalar1=s1, scalar2=None, op0=op0)

                def f_rs(o, i):
                    return lambda e: e.reduce_sum(out=o, in_=i, axis=AXX)

                def f_cpd(o, i):
                    return lambda e: e.tensor_copy(out=o, in_=i)

                def f_iota(o, pat, cm):
                    return lambda e: e.iota(o, pattern=pat, base=0, channel_multiplier=cm)
                r_md = Res("md")
                Mk = AR.alloc([32, 8], F32); PA = AR.alloc([32, 8], F32); PBt = AR.alloc([32, 8], F32)
                rank = AR.alloc([32, 8], F32); cum = AR.alloc([32, 8], F32); F1 = AR.alloc([32, 8], F32); F2 = AR.alloc([32, 8], F32)
                tmp3 = AR.alloc([32, 8], F32)
                totb = AR.alloc([8], BF16); ustr = AR.alloc([128], BF16); uones = AR.alloc([128], BF16); ustf = AR.alloc([128], F32)
                offn = AR.alloc([2, 8], F32)
                thr_i = AR.alloc([9], I32); thr = AR.alloc([9], F32); cmp9 = AR.alloc([8, 9], F32)
                cnt = AR.alloc([8], F32); Gs = AR.alloc([8], F32); ends = AR.alloc([8], F32); sbs = AR.alloc([8], F32)
                gio_i = AR.alloc([NGRP], I32); gio = AR.alloc([NGRP], F32); cmp2 = AR.alloc([NGRP, 8], F32); ge = AR.alloc([NGRP], F32)
                kp_i = AR.alloc([8], I32); kp = AR.alloc([8], F32); jp_i = AR.alloc([28], I32); jp = AR.alloc([28], F32)
                geg = AR.alloc([NGRP], F32); ged = AR.alloc([NGRP], F32)
                idxGUf = AR.alloc([NGRP, 8], F32); idxGU = AR.alloc([NGRP, 8], I32)
                idxDf = AR.alloc([NGRP, 28], F32); idxD = AR.alloc([NGRP, 28], I32)
                d12f = AR.alloc([2, 32], F32)

                def MD(eng, fn, extra_r=()):
                    S.op(eng, fn, reads=[r_md] + list(extra_r), writes=[r_md])
                MD("dve", f_tsc(Mk, Gall[:], 0.0, ALU.is_gt), [r_Gall])
                MD("dve", f_cpd(PA, Mk))
                src_, dst_ = PA, PBt
                for sft in (1, 2, 4, 8, 16):
                    MD("dve", f_cpd(dst_[:, 0:sft, :], src_[:, 0:sft, :]))
                    MD("dve", f_tt(dst_[:, sft:32, :], src_[:, sft:32, :], src_[:, 0:32 - sft, :], ALU.add))
                    src_, dst_ = dst_, src_
                incl = src_
                MD("dve", f_cpd(totb, incl[:, 31, :]))
                MD("pool", f_ms(ustf, 1.0))
                MD("pool", lambda e: e.affine_select(out=ustf, in_=ustf, pattern=[[1, 128]], compare_op=ALU.is_gt, fill=0.0, base=0, channel_multiplier=-1))
                MD("dve", f_cpd(ustr, ustf))
                MD("pool", f_ms(uones, 1.0))
                pbm, r_pbm = PF.next()
                S.op("pe", [f_mm(pbm[:, 0:8], ustr, totb, True, True), f_mm(pbm[:, 8:16], uones, totb, True, True)], reads=[r_md], writes=[r_pbm])
                S.op("dve", f_cpd(offn, pbm[:, 0:16].rearrange("p (a e) -> p a e", e=8)), reads=[r_pbm, r_md], writes=[r_md])
                MD("dve", f_tt(rank, incl, Mk, ALU.subtract))
                MD("dve", f_tt(rank, rank, offn[:, 0:1, :].to_broadcast([128, 32, 8]), ALU.add))
                MD("pool", f_iota(thr_i, [[512, 9]], 0))
                MD("dve", f_cpd(thr, thr_i))
                MD("dve", f_tt(cmp9, offn[:, 1, :].unsqueeze(2).to_broadcast([128, 8, 9]), thr.unsqueeze(1).to_broadcast([128, 8, 9]), ALU.is_gt))
                MD("dve", f_rs(cnt, cmp9))
                MD("pool", f_ms(Gs[:, 0:1], 0.0))
                for e_ in range(1, 8):
                    MD("dve", f_tt(Gs[:, e_:e_ + 1], Gs[:, e_ - 1:e_], cnt[:, e_ - 1:e_], ALU.add))
                MD("dve", f_tt(ends, Gs, cnt, ALU.add))
                MD("dve", f_tsc(sbs, Gs, 512.0, ALU.mult))
                MD("dve", f_tt(rank, rank, sbs.unsqueeze(1).to_broadcast([128, 32, 8]), ALU.add))
                MD("pool", f_ms(cum[:, :, 0:1], 0.0))
                for e_ in range(1, 8):
                    MD("dve", f_tt(cum[:, :, e_:e_ + 1], cum[:, :, e_ - 1:e_], Mk[:, :, e_ - 1:e_], ALU.add))
                MD("dve", f_stt(F1, cum, 0.0, Mk, ALU.is_equal, ALU.mult))
                MD("dve", f_stt(F2, cum, 1.0, Mk, ALU.is_equal, ALU.mult))
                for ki, Fk in enumerate((F1, F2)):
                    MD("dve", f_tt(tmp3, Fk, rank, ALU.mult))
                    MD("dve", f_rs(d12f[:, ki, :], tmp3))
                    MD("dve", f_tt(tmp3, Fk, Gall[:], ALU.mult), [r_Gall])
                    S.op("dve", f_rs(g12[:, ki, :], tmp3), reads=[r_md], writes=[r_md, r_route])
                S.op("dve", f_cpd(d12i[:], d12f), reads=[r_md], writes=[r_md, r_route])
                MD("pool", f_iota(gio_i, [[1, NGRP]], 0))
                MD("dve", f_cpd(gio, gio_i))
                MD("dve", f_tt(cmp2, ends.unsqueeze(1).to_broadcast([128, NGRP, 8]), gio.unsqueeze(2).to_broadcast([128, NGRP, 8]), ALU.is_le))
                MD("dve", f_rs(ge, cmp2))
                MD("dve", lambda e: e.tensor_scalar_min(out=ge, in0=ge, scalar1=7.0))
                MD("pool", f_iota(kp_i, [[128, 8]], 1))
                MD("dve", f_cpd(kp, kp_i))
                MD("pool", f_iota(jp_i, [[128, 28]], 1))
                MD("dve", f_cpd(jp, jp_i))
                MD("dve", f_tsc(geg, ge, float(D), ALU.mult))
                MD("dve", f_tsc(ged, ge, 3584.0, ALU.mult))
                MD("dve", f_tt(idxGUf, geg.unsqueeze(2).to_broadcast([128, NGRP, 8]), kp.unsqueeze(1).to_broadcast([128, NGRP, 8]), ALU.add))
                MD("dve", f_cpd(idxGU, idxGUf))
                MD("dve", f_tt(idxDf, ged.unsqueeze(2).to_broadcast([128, NGRP, 28]), jp.unsqueeze(1).to_broadcast([128, NGRP, 28]), ALU.add))
                MD("dve", f_cpd(idxD, idxDf))

                r_Hs = Res("Hs", multi=True); r_Ys = Res("Ys", multi=True)
                hts = Ring([(AR.alloc([D], BF16), Res("ht%d" % i)) for i in range(3)])

                def f_scatter(dst, idx_ap, src):
                    return lambda e: e.indirect_dma_start(out=dst, out_offset=bass.IndirectOffsetOnAxis(ap=idx_ap, axis=0), in_=src, in_offset=None)

                def f_gather(dst, src, idx_ap):
                    return lambda e: e.indirect_dma_start(out=dst, out_offset=None, in_=src, in_offset=bass.IndirectOffsetOnAxis(ap=idx_ap, axis=0))
                for a in range(32):
                    ht, r_ht = hts.next()
                    S.dma("sp", ht, h2tok[a * 128:(a + 1) * 128, :], reads=[r_h2tok], writes=[r_ht])
                    for ki in range(2):
                        S.dma_fn("pool", f_scatter(Hs[:, :], d12i[:, ki, a:a + 1].bitcast(U32), ht), reads=[r_ht, r_route], writes=[r_Hs])

                hss = Ring([(AR.alloc([D], BF16), Res("hs%d" % i)) for i in range(2)])
                hTg = Ring([(AR.alloc([8, 512], BF16), Res("hTg%d" % i, multi=True)) for i in range(2)])
                wgus = Ring([(AR.alloc([8, 1792], BF16), Res("wgu%d" % i, multi=True)) for i in range(2)])
                wdqs = Ring([(AR.alloc([7, D], BF16), Res("wdq%d" % i, multi=True)) for i in range(2)])
                aTqs = Ring([(AR.alloc([7, 512], BF16), Res("aTq%d" % i, multi=True)) for i in range(2)])
                sgs = Ring([(AR.alloc([512], BF16), Res("sg%d" % i)) for i in range(3)])
                accs = Ring([(AR.alloc([4, D], F32), Res("acc%d" % i, multi=True)) for i in range(1)])
                mwd2 = mwd.rearrange("e f n -> (e f) n")
                for g in range(NGRP):
                    hT, r_hT = hTg.next()
                    for tt in range(4):
                        hs_, r_hs = hss.next()
                        S.dma("sp", hs_, Hs[g * 512 + tt * 128:g * 512 + (tt + 1) * 128, :], reads=[r_Hs], writes=[r_hs])
                        transpose_to(hs_, r_hs, hT[:, :, tt * 128:(tt + 1) * 128], r_hT, 8)
                    acc, r_acc = accs.next()
                    for q in range(4):
                        wgu, r_wgu = wgus.next(); wdq, r_wdq = wdqs.next(); aTq, r_aTq = aTqs.next()
                        for k in range(8):
                            S.dma_fn("pool", f_gather(wgu[:, k, :], mwgu[q][:, :], idxGU[:, g, k:k + 1].bitcast(U32)), reads=[r_mw, r_md], writes=[r_wgu])
                        for j in range(7):
                            S.dma_fn("pool", f_gather(wdq[:, j, :], mwd2, idxD[:, g, q * 7 + j:q * 7 + j + 1].bitcast(U32)), reads=[r_mw, r_md], writes=[r_wdq])
                        for c in range(7):
                            pg, r_pg = PF.next(); pu, r_pu = PF.next()
                            S.op("pe", [f_mm(pg[:, :], wgu[:, k, c * 128:(c + 1) * 128], hT[:, k, :], k == 0, k == 7) for k in range(8)], reads=[r_wgu, r_hT], writes=[r_pg])
                            S.op("pe", [f_mm(pu[:, :], wgu[:, k, 896 + c * 128:896 + (c + 1) * 128], hT[:, k, :], k == 0, k == 7) for k in range(8)], reads=[r_wgu, r_hT], writes=[r_pu])
                            sg, r_sg = sgs.next()
                            S.op("act", f_act(sg, pg[:, :], AF.Silu), reads=[r_pg], writes=[r_sg])
                            S.op("dve", f_tt(aTq[:, c, :], pu[:, :], sg, ALU.mult), reads=[r_pu, r_sg], writes=[r_aTq])
                        for tt in range(4):
                            for nh in range(2):
                                pd, r_pd = PF.next()
                                S.op("pe", [f_mm(pd[:, :], aTq[:, c, tt * 128:(tt + 1) * 128], wdq[:, c, nh * 512:(nh + 1) * 512], c == 0, c == 6) for c in range(7)],
                                     reads=[r_aTq, r_wdq], writes=[r_pd])
                                ao = acc[:, tt, nh * 512:(nh + 1) * 512]
                                if q == 0:
                                    copy_op(evac_eng(), ao, pd[:, :], [r_pd], [r_acc])
                                else:
                                    S.op("dve", f_tt(ao, pd[:, :], ao, ALU.add), reads=[r_pd, r_acc], writes=[r_acc])
                    for tt in range(4):
                        S.dma("sp", Ys[g * 512 + tt * 128:g * 512 + (tt + 1) * 128, :], acc[:, tt, :], reads=[r_acc], writes=[r_Ys])

                S.barrier(); AR.reset()
                g2t = AR.alloc([D], F32); r_g2t = Res("g2t")
                S.dma("sp", g2t, modd[0:1, 5 * D:6 * D].partition_broadcast(128), reads=[R["modd"]], writes=[r_g2t])
                fg = AR.alloc([D], F32); r_fg = Res("fg")
                S.dma("sp", fg, final_g[0:1, :].partition_broadcast(128), writes=[r_fg])
                y1s = Ring([(AR.alloc([D], F32), Res("y1%d" % i)) for i in range(2)])
                y2s = Ring([(AR.alloc([D], F32), Res("y2%d" % i)) for i in range(2)])
                xm5 = Ring([(AR.alloc([D], F32), Res("xm5%d" % i)) for i in range(2)])
                xo5 = Ring([(AR.alloc([D], F32), Res("xo5%d" % i)) for i in range(2)])
                tmp6 = AR.alloc([D], F32); r_tmp6 = Res("tmp6")
                ss6 = AR.alloc([1], F32); r_ss6 = Res("ss6")
                for a in range(32):
                    tok0 = PADR + a * 128
                    y1, r_y1 = y1s.next(); y2, r_y2 = y2s.next()
                    S.dma_fn("pool", f_gather(y1, Ys[:, :], d12i[:, 0, a:a + 1].bitcast(U32)), reads=[r_Ys, r_route], writes=[r_y1])
                    S.dma_fn("pool", f_gather(y2, Ys[:, :], d12i[:, 1, a:a + 1].bitcast(U32)), reads=[r_Ys, r_route], writes=[r_y2])
                    xm, r_xm = xm5.next(); xo, r_xo = xo5.next()
                    S.dma("sp", xm, xa[tok0:tok0 + 128, :], reads=[r_xa], writes=[r_xm])
                    S.op("dve", f_ts(xo, y1, g12[:, 0, a:a + 1], ALU.mult), reads=[r_y1, r_route], writes=[r_xo])
                    S.op("dve", f_stt(xo, y2, g12[:, 1, a:a + 1], xo, ALU.mult, ALU.add), reads=[r_y2, r_route, r_xo], writes=[r_xo])
                    S.op("pool", f_tt(xo, xo, g2t, ALU.mult), reads=[r_xo, r_g2t], writes=[r_xo])
                    S.op("pool", f_tt(xo, xo, xm, ALU.add), reads=[r_xo, r_xm], writes=[r_xo])
                    S.op("act", f_act(tmp6, xo, AF.Square, accum_out=ss6), reads=[r_xo], writes=[r_tmp6, r_ss6])
                    S.op("act", f_act(ss6, ss6, AF.Sqrt, bias=epsb[:], scale=1.0 / D), reads=[r_ss6, r_const], writes=[r_ss6])
                    S.op("dve", f_rec(ss6, ss6), reads=[r_ss6], writes=[r_ss6])
                    S.op("dve", f_stt(xo, xo, ss6, fg, ALU.mult, ALU.mult), reads=[r_xo, r_ss6, r_fg], writes=[r_xo])
                    S.dma("sp", y_out[a * 128:(a + 1) * 128, :], xo, reads=[r_xo])
                continue
            nexp = 8 if last else 1
            nch = 28 if last else 22
            BL = 2
            nblk = nch // BL
            gt2s = []
            for (t0, tl, row) in seqs5:
                g_ = AR.alloc([D], F32); r_ = Res("gt2_%d" % row)
                S.dma("sp", g_, modd[row:row + 1, 5 * D:6 * D].partition_broadcast(128), reads=[R["modd"]], writes=[r_])
                gt2s.append((g_, r_))
            if last:
                fg = AR.alloc([D], F32); r_fg = Res("fg")
                S.dma("sp", fg, final_g[0:1, :].partition_broadcast(128), writes=[r_fg])
            h2g = Ring([(AR.alloc([8, 512], BF16), Res("h2g%d" % i)) for i in range(2)])
            wgs = Ring([(AR.alloc([8, BL * 128], BF16), Res("wgs%d" % i)) for i in range(3)])
            wus = Ring([(AR.alloc([8, BL * 128], BF16), Res("wus%d" % i)) for i in range(3)])
            wds = [(AR.alloc([BL, D], BF16), Res("wds%d" % i)) for i in range(nblk)]
            aTb = AR.alloc([nch, 512], BF16); r_aT5 = Res("aT5", multi=True)
            sgs = Ring([(AR.alloc([512], BF16), Res("sg%d" % i)) for i in range(3)])
            accs = Ring([(AR.alloc([4, D], F32), Res("acc%d" % i, multi=True)) for i in range(1)])
            gts = Ring([(AR.alloc([4, 8], F32), Res("gts%d" % i)) for i in range(2)])
            xm5 = Ring([(AR.alloc([D], F32), Res("xm5%d" % i)) for i in range(2)])
            xo5 = Ring([(AR.alloc([D], F32), Res("xo5%d" % i)) for i in range(2)])
            tmp6 = AR.alloc([D], F32); r_tmp6 = Res("tmp6")
            ss6 = AR.alloc([1], F32); r_ss6 = Res("ss6")
            r_wsrc = r_mw if last else r_fw
            gtl = r_gtl = None
            for (g0, gs, si) in groups5:
                if True:
                    g2t, r_g2t = gt2s[si]
                    hg, r_hg = h2g.next()
                    S.dma("sp", hg[:, :, 0:gs], h2Td[:, g0:g0 + gs].rearrange("(k p) t -> p k t", p=128), reads=[R["h2T"]], writes=[r_hg])
                    acc, r_acc = accs.next()
                    if last:
                        gtl, r_gtl = gts.next()
                        S.dma("sp", gtl[:, 0:gs // 128, :], gatesd[g0:g0 + gs, :].rearrange("(a p) e -> p a e", p=128), reads=[R["gates"]], writes=[r_gtl])
                    for ex in range(nexp):
                        if last:
                            Wg, Wu, Wd = mwg[ex], mwu[ex], mwd[ex]
                        else:
                            Wg, Wu, Wd = fwg, fwu, fwd
                        for b in range(nblk):
                            f0 = b * BL * 128
                            wg_, r_wg = wgs.next(); wu_, r_wu = wus.next(); wd_, r_wd = wds[b]
                            S.dma("sp", wg_, Wg[b].rearrange("p (k f) -> p k f", k=8), reads=[r_wsrc], writes=[r_wg])
                            S.dma("sp", wu_, Wu[b].rearrange("p (k f) -> p k f", k=8), reads=[r_wsrc], writes=[r_wu])
                            S.dma("pool", wd_, Wd[f0:f0 + BL * 128, :].rearrange("(j p) n -> p j n", p=128), reads=[r_wsrc], writes=[r_wd])
                            for j in range(BL):
                                ch = b * BL + j
                                pg, r_pg = PF.next(); pu, r_pu = PF.next()
                                S.op("pe", [f_mm(pg[:, 0:gs], wg_[:, k, j * 128:(j + 1) * 128], hg[:, k, 0:gs], k == 0, k == 7) for k in range(8)],
                                     reads=[r_wg, r_hg], writes=[r_pg])
                                S.op("pe", [f_mm(pu[:, 0:gs], wu_[:, k, j * 128:(j + 1) * 128], hg[:, k, 0:gs], k == 0, k == 7) for k in range(8)],
                                     reads=[r_wu, r_hg], writes=[r_pu])
                                sg, r_sg = sgs.next()
                                S.op("act", f_act(sg[:, 0:gs], pg[:, 0:gs], AF.Silu), reads=[r_pg], writes=[r_sg])
                                S.op("dve", f_tt(aTb[:, ch, 0:gs], pu[:, 0:gs], sg[:, 0:gs], ALU.mult), reads=[r_pu, r_sg], writes=[r_aT5])
                        for tt in range(gs // 128):
                            for nh in range(2):
                                pd, r_pd = PF.next()
                                S.op("pe", [f_mm(pd[:, :], aTb[:, ch, tt * 128:(tt + 1) * 128], wds[ch // BL][0][:, ch % BL, nh * 512:(nh + 1) * 512], ch == 0, ch == nch - 1) for ch in range(nch)],
                                     reads=[r_aT5] + [w_[1] for w_ in wds], writes=[r_pd])
                                ao = acc[:, tt, nh * 512:(nh + 1) * 512]
                                if not last:
                                    copy_op(evac_eng(), ao, pd[:, :], [r_pd], [r_acc])
                                elif ex == 0:
                                    S.op("dve", f_ts(ao, pd[:, :], gtl[:, tt, ex:ex + 1], ALU.mult), reads=[r_pd, r_gtl], writes=[r_acc])
                                else:
                                    S.op("dve", f_stt(ao, pd[:, :], gtl[:, tt, ex:ex + 1], ao, ALU.mult, ALU.add), reads=[r_pd, r_gtl, r_acc], writes=[r_acc])
                    for tt in range(gs // 128):
                        tok0 = g0 + tt * 128
                        xm, r_xm = xm5.next(); xo, r_xo = xo5.next()
                        S.dma("sp", xm, xa[tok0:tok0 + 128, :], reads=[r_xa], writes=[r_xm])
                        S.op("dve", f_tt(xo, acc[:, tt, :], g2t, ALU.mult), reads=[r_acc, r_g2t], writes=[r_xo])
                        S.op("pool", f_tt(xo, xo, xm, ALU.add), reads=[r_xo, r_xm], writes=[r_xo])
                        if not last:
                            S.dma("sp", xb[xbrow(tok0):xbrow(tok0) + 128, :], xo, reads=[r_xo], writes=[r_xb])
                            if dbg:
                                S.dma("sp", dbgs["xb1"][xbrow(tok0):xbrow(tok0) + 128, :], xo, reads=[r_xo])
                        else:
                            S.op("act", f_act(tmp6, xo, AF.Square, accum_out=ss6), reads=[r_xo], writes=[r_tmp6, r_ss6])
                            S.op("act", f_act(ss6, ss6, AF.Sqrt, bias=epsb[:], scale=1.0 / D), reads=[r_ss6, r_const], writes=[r_ss6])
                            S.op("dve", f_rec(ss6, ss6), reads=[r_ss6], writes=[r_ss6])
                            S.op("dve", f_stt(xo, xo, ss6, fg, ALU.mult, ALU.mult), reads=[r_xo, r_ss6, r_fg], writes=[r_xo])
                            S.dma("sp", y_out[tok0 - PADR:tok0 - PADR + 128, :], xo, reads=[r_xo])
            if dbg and not last:
                for i in range(66):
                    S.dma("sp", dbgs["xa0"][i * 128:(i + 1) * 128, :], xa[i * 128:(i + 1) * 128, :], reads=[r_xa])
        except _StopBuild:
            pass
        S.barrier()
        for dn, dst_ in dump_out.items():
            src_ = scr_by_name[dn]
            if len(src_.shape) == 3:
                for i_ in range(src_.shape[0]):
                    S.dma("sp", dst_[i_], src_[i_])
            else:
                n0 = src_.shape[0]
                step = max(1, n0 // 8)
                for i_ in range(0, n0, step):
                    S.dma("sp", dst_[i_:min(n0, i_ + step)], src_[i_:min(n0, i_ + step)])
        S.barrier()
        S.emit()
    return nc


def _bf16(a):
    return np.asarray(a, dtype=np.float32).astype(ml_dtypes.bfloat16)


def _fill_bias(out_v, rpb_l, r0, krow0, nch):
    cs = np.clip(np.arange(64) - 8, 0, 48)
    for qi in range(2):
        r = r0 + qi
        start = min(max(r - 4, 0), 120)
        for j in range(nch):
            for kp in range(2):
                krow = krow0 + 2 * j + kp
                if not (start <= krow < start + 8):
                    continue
                dr = krow - r + 7
                for c in range(64):
                    kcs = np.arange(cs[c], cs[c] + 16)
                    dc = kcs - c + 15
                    out_v[kp * 64 + kcs, :, j, qi * 64 + c] = rpb_l[:, dr, dc].T


def _bias_tables_l0(rpb_l):
    out = np.full((5, 128, 8, 5, 128), NEG, np.float32)
    for v, m in {0: 4, 1: 0, 2: 1, 3: 62, 4: 63}.items():
        r0 = 2 * m
        _fill_bias(out[v], rpb_l, r0, min(max(r0 - 4, 0), 118), 5)
    return out.reshape(5, 128, 8 * 5 * 128)


def _bias_tables_l1(rpb_l, h):
    out = np.full((5, 128, 8, 6, 128), NEG, np.float32)
    for v, ml in {0: 8, 1: 0, 2: 1, 3: 30, 4: 31}.items():
        r0 = 2 * (ml + 32 * h)
        _fill_bias(out[v], rpb_l, r0, r0 - (6 if ml == 31 else 4), 6)
    return out.reshape(5, 128, 8 * 6 * 128)


def _tables():
    Ls = 8192
    l1 = np.arange(128)[:, None, None]
    l0 = np.arange(64)[None, :, None]
    k1 = np.arange(128)[None, None, :]
    ang = 2 * np.pi * ((k1 * (64 * l1 + l0)) % Ls) / Ls
    sc = 1.0 / np.sqrt(Ls)
    dftA = np.stack([np.cos(ang) * sc, -np.sin(ang) * sc], axis=2)
    a64 = 2 * np.pi * (np.arange(64)[:, None] * np.arange(64)[None, :] % 64) / 64
    dft64 = np.stack([np.cos(a64), np.sin(a64), -np.sin(a64)], axis=1)
    a256 = 2 * np.pi * (np.arange(256)[:, None] * np.arange(256)[None, :] % 256) / 256
    dftc = np.stack([np.cos(a256) / 16.0, -np.sin(a256) / 16.0], axis=1)
    cb = np.zeros((256, 2, 256), np.float64)
    for g in range(4):
        cb[g * 64:(g + 1) * 64, 0, g * 64:(g + 1) * 64] = np.cos(a64) / 8.0
        cb[g * 64:(g + 1) * 64, 1, g * 64:(g + 1) * 64] = np.sin(a64) / 8.0
    return (_bf16(dftA.reshape(128, -1)), _bf16(dft64.reshape(64, -1)), _bf16(dftc.reshape(256, -1)), _bf16(cb.reshape(256, -1)))


_NC_CACHE = {}


def kernel(x, c, ctx, c_ctx, norm1_g, norm2_g, w_ada, b_ada, w_in, conv_w, na_rpb, w_fourier, w_na, w_conv_out,
           w_out, ffn_w_gate, ffn_w_up, ffn_w_down, moe_router, moe_w_gate, moe_w_up, moe_w_down, final_g, _dbg=False, _stop=None, _dumps=(), _nb=None):
    f = lambda a: np.ascontiguousarray(np.asarray(a, dtype=np.float32))
    x = f(x); c = f(c); ctx = f(ctx); c_ctx = f(c_ctx)
    B = x.shape[0]
    dftA, dft64, dftc, c64bd = _tables()
    rpb = f(na_rpb)
    biasT = _bf16(_bias_tables_l0(rpb[0]))
    biasT1 = [_bf16(_bias_tables_l1(rpb[1], h)) for h in range(2)]
    emask = [np.array([[0.0, 1.0]], np.float32), np.array([[1.0, 0.0]], np.float32)]
    shared = {
        "norm1_g": f(norm1_g), "norm2_g": f(norm2_g), "w_ada": f(w_ada), "b_ada": f(b_ada), "w_in": f(w_in),
        "conv_w": f(conv_w), "w_fourier": f(w_fourier), "w_na": f(w_na), "w_conv_out": f(w_conv_out), "w_out": f(w_out),
        "ffn_w_gate": f(ffn_w_gate), "ffn_w_up": f(ffn_w_up), "ffn_w_down": f(ffn_w_down), "moe_router": f(moe_router),
        "moe_w_gate": f(moe_w_gate), "moe_w_up": f(moe_w_up), "moe_w_down": f(moe_w_down), "final_g": f(final_g).reshape(1, D),
        "biasT": biasT, "dftA": dftA, "dft64": dft64, "dftc": dftc, "c64bd": c64bd,
    }
    key = (bool(_dbg), _stop, tuple(_dumps))
    if key not in _NC_CACHE:
        _NC_CACHE[key] = build_program(dbg=bool(_dbg), stop=_stop, dumps=tuple(_dumps))
    nc = _NC_CACHE[key]
    if _nb is not None:
        B = _nb
    in_maps = []
    for b in range(B):
        for h in range(2):
            m = dict(shared)
            m["x"] = x[b]
            m["ctx"] = ctx[b]
            m["cvec"] = np.ascontiguousarray(np.stack([c[b], c_ctx], axis=0))
            m["biasT1"] = biasT1[h]
            m["edgemask"] = emask[h]
            in_maps.append(m)
    res = run_bass_kernel_spmd(nc, in_maps, core_ids=list(range(2 * B)))
    out = np.stack([np.concatenate([np.asarray(res.results[2 * b + h]["y"], dtype=np.float32) for h in range(2)], axis=0) for b in range(B)], axis=0)
    if _dbg or _dumps:
        return out, res
    return out
```
